# Optimizing a Trainium2 kernel written in Bass

```python
import jax, jax.numpy as jnp
from jax import lax
import numpy as np

D_MODEL = 1024
BATCH = 2
SEQ = 8192
DEPTH = 4

N_MIXERS = 3
CHUNK = 128
GM_WIDTH = 2 * D_MODEL
GM_GROUPS = 8
GM_GROUP_DIM = GM_WIDTH // GM_GROUPS
FOX_HEADS = 16
FOX_HEAD_DIM = D_MODEL // FOX_HEADS
Q_BLOCK = 128
POOL_WINDOWS = (2, 4, 8, 16)
POOL_GROUPS = len(POOL_WINDOWS)
POOL_WIDTH = D_MODEL
POOL_GROUP_DIM = POOL_WIDTH // POOL_GROUPS
D_FF = 7 * D_MODEL // 2
N_EXPERTS = 8
TOP_K = 2
EXPERT_BLOCK = 512
DN_ALPHA = (2 * DEPTH) ** 0.25
DN_BETA = (8 * DEPTH) ** -0.25
LN_EPS = 1e-5
MOD_SCALE = 0.25

N_GM = (DEPTH + 2) // 3
N_FOX = (DEPTH + 1) // 3
N_POOL = DEPTH // 3
N_DENSE = (DEPTH + 1) // 2
N_MOE = DEPTH // 2

kernel_name = 'hybrid_gmlp_fox_pool_moe_deepnorm_adaln'


def layer_norm(x, g, b):
    xf = x.astype(jnp.float32)
    mu = jnp.mean(xf, axis=-1, keepdims=True)
    var = jnp.mean(jnp.square(xf - mu), axis=-1, keepdims=True)
    y = (xf - mu) * lax.rsqrt(var + LN_EPS)
    return (y * g.astype(jnp.float32) + b.astype(jnp.float32)).astype(x.dtype)


def gmlp_chunk_mixer(h, w_in, ln_g, ln_b, w_s, b_s, w_out):
    B_, S_, _ = h.shape
    z = jax.nn.gelu(h @ w_in, approximate=False)
    u, v = jnp.split(z, 2, axis=-1)
    v = layer_norm(v, ln_g, ln_b)
    v = v.reshape(B_, S_ // CHUNK, CHUNK, GM_GROUPS, GM_GROUP_DIM)
    causal = jnp.tril(jnp.ones((CHUNK, CHUNK), dtype=bool))
    w = jnp.where(causal[None], w_s, 0).astype(v.dtype)
    mixed = jnp.einsum('gts,bnsgc->bntgc', w, v) + b_s.T[:, :, None].astype(v.dtype)
    mixed = mixed.reshape(B_, S_, GM_WIDTH)
    return (u * mixed) @ w_out


def forgetting_attention(h, w_in, b_f, w_out):
    B_, S_, _ = h.shape
    proj = h @ w_in
    q, k, v, f = jnp.split(proj, [D_MODEL, 2 * D_MODEL, 3 * D_MODEL], axis=-1)

    def to_heads(t):
        return t.reshape(B_, S_, FOX_HEADS, FOX_HEAD_DIM).transpose(0, 2, 1, 3)

    q, k, v = to_heads(q), to_heads(k), to_heads(v)
    log_f = jax.nn.log_sigmoid((f + b_f).astype(jnp.float32))
    cum = jnp.cumsum(log_f, axis=1).transpose(0, 2, 1)
    scale = FOX_HEAD_DIM ** -0.5
    key_pos = jnp.arange(S_)

    def query_block(i):
        start = i * Q_BLOCK
        qb = lax.dynamic_slice_in_dim(q, start, Q_BLOCK, axis=2)
        cq = lax.dynamic_slice_in_dim(cum, start, Q_BLOCK, axis=2)
        s = jnp.einsum('bhqd,bhkd->bhqk', qb, k).astype(jnp.float32) * scale
        s = s + cq[..., :, None] - cum[..., None, :]
        q_pos = start + jnp.arange(Q_BLOCK)
        mask = key_pos[None, :] <= q_pos[:, None]
        p = jax.nn.softmax(jnp.where(mask, s, -jnp.inf), axis=-1)
        return jnp.einsum('bhqk,bhkd->bhqd', p.astype(v.dtype), v)

    out = lax.map(query_block, jnp.arange(S_ // Q_BLOCK))
    out = out.transpose(1, 0, 3, 2, 4).reshape(B_, S_, D_MODEL)
    return out @ w_out


def multiscale_pool_mixer(h, w_in, w_grp, layer_scale, w_out):
    B_, S_, _ = h.shape
    z = h @ w_in
    zf = z.astype(jnp.float32)
    cs = jnp.pad(jnp.cumsum(zf, axis=1), ((0, 0), (1, 0), (0, 0)))
    pos = jnp.arange(S_)
    outs = []
    for g, win in enumerate(POOL_WINDOWS):
        lo_c, hi_c = g * POOL_GROUP_DIM, (g + 1) * POOL_GROUP_DIM
        cs_g = cs[..., lo_c:hi_c]
        lo = jnp.maximum(pos + 1 - win, 0)
        window_sum = cs_g[:, 1:, :] - cs_g[:, lo, :]
        count = jnp.minimum(pos + 1, win).astype(jnp.float32)
        pooled = window_sum / count[None, :, None] - zf[..., lo_c:hi_c]
        outs.append(jnp.einsum('bsc,cd->bsd', pooled.astype(z.dtype), w_grp[g]))
    y = jnp.concatenate(outs, axis=-1) * layer_scale
    return y @ w_out


def swiglu(h, w13, w2):
    a, b = jnp.split(h @ w13, 2, axis=-1)
    return (jax.nn.silu(a) * b) @ w2


def moe_swiglu(h, w_router, b_router, w13, w2):
    B_, S_, D = h.shape
    xt = h.reshape(-1, D)
    T = xt.shape[0]
    logits = (xt @ w_router).astype(jnp.float32) + b_router.astype(jnp.float32)
    top_val, top_idx = lax.top_k(logits, TOP_K)
    gates = jax.nn.softmax(top_val, axis=-1)
    n_assign = T * TOP_K
    e_flat = top_idx.reshape(-1).astype(jnp.int32)
    tok_flat = jnp.repeat(jnp.arange(T, dtype=jnp.int32), TOP_K)
    g_flat = gates.reshape(-1)
    order = jnp.argsort(e_flat * n_assign + jnp.arange(n_assign, dtype=jnp.int32))
    e_sorted, tok_sorted, g_sorted = e_flat[order], tok_flat[order], g_flat[order]
    counts = jnp.bincount(e_flat, length=N_EXPERTS)
    padded = (counts + EXPERT_BLOCK - 1) // EXPERT_BLOCK * EXPERT_BLOCK
    pad_end = jnp.cumsum(padded)
    pad_start = pad_end - padded
    raw_start = jnp.cumsum(counts) - counts
    dest = pad_start[e_sorted] + (jnp.arange(n_assign) - raw_start[e_sorted])
    capacity = (n_assign + EXPERT_BLOCK - 1) // EXPERT_BLOCK * EXPERT_BLOCK + N_EXPERTS * EXPERT_BLOCK
    n_blocks = capacity // EXPERT_BLOCK
    slot_tok = jnp.full((capacity,), T, dtype=jnp.int32).at[dest].set(tok_sorted)
    x_pad = jnp.concatenate([xt, jnp.zeros((1, D), xt.dtype)], axis=0)
    x_slots = x_pad[slot_tok].reshape(n_blocks, EXPERT_BLOCK, D)
    block_expert = jnp.minimum(
        jnp.searchsorted(pad_end, jnp.arange(n_blocks) * EXPERT_BLOCK, side='right'),
        N_EXPERTS - 1)

    def expert_block(args):
        xb, e = args
        return swiglu(xb, w13[e], w2[e])

    y_slots = lax.map(expert_block, (x_slots, block_expert)).reshape(capacity, D)
    y = jnp.zeros_like(xt).at[tok_sorted].add(y_slots[dest] * g_sorted[:, None].astype(xt.dtype))
    return y.reshape(B_, S_, D)


def setup_inputs(seed: int = 0) -> dict:
    key = jax.random.key(seed)
    ks = jax.random.split(key, 24)
    nrm = jax.random.normal
    D, F, E = D_MODEL, D_FF, N_EXPERTS
    return {
        'x': nrm(ks[0], (BATCH, SEQ, D), jnp.float32),
        'c': nrm(ks[1], (BATCH, D), jnp.float32),
        'mod_w': nrm(ks[2], (DEPTH, D, 6 * D), jnp.float32) * (MOD_SCALE * D ** -0.5),
        'mod_b': nrm(ks[3], (DEPTH, 6 * D), jnp.float32) * 0.02,
        'ln_g': 1.0 + 0.02 * nrm(ks[4], (DEPTH, 2, D), jnp.float32),
        'ln_b': 0.02 * nrm(ks[5], (DEPTH, 2, D), jnp.float32),
        'gm_w_in': nrm(ks[6], (N_GM, D, 2 * GM_WIDTH), jnp.float32) * D ** -0.5,
        'gm_ln_g': 1.0 + 0.02 * nrm(ks[7], (N_GM, GM_WIDTH), jnp.float32),
        'gm_ln_b': 0.02 * nrm(ks[8], (N_GM, GM_WIDTH), jnp.float32),
        'gm_w_s': nrm(ks[9], (N_GM, GM_GROUPS, CHUNK, CHUNK), jnp.float32) * CHUNK ** -0.5,
        'gm_b_s': 1.0 + 0.1 * nrm(ks[10], (N_GM, GM_GROUPS, CHUNK), jnp.float32),
        'gm_w_out': nrm(ks[11], (N_GM, GM_WIDTH, D), jnp.float32) * (DN_BETA * GM_WIDTH ** -0.5),
        'fox_w_in': nrm(ks[12], (N_FOX, D, 3 * D + FOX_HEADS), jnp.float32) * D ** -0.5,
        'fox_b_f': jax.random.uniform(ks[13], (N_FOX, FOX_HEADS), jnp.float32, 1.0, 4.0),
        'fox_w_out': nrm(ks[14], (N_FOX, D, D), jnp.float32) * (DN_BETA * D ** -0.5),
        'pool_w_in': nrm(ks[15], (N_POOL, D, POOL_WIDTH), jnp.float32) * D ** -0.5,
        'pool_w_grp': nrm(ks[16], (N_POOL, POOL_GROUPS, POOL_GROUP_DIM, POOL_GROUP_DIM), jnp.float32) * POOL_GROUP_DIM ** -0.5,
        'pool_scale': 1.0 + 0.1 * nrm(ks[17], (N_POOL, POOL_WIDTH), jnp.float32),
        'pool_w_out': nrm(ks[18], (N_POOL, POOL_WIDTH, D), jnp.float32) * (DN_BETA * POOL_WIDTH ** -0.5),
        'ffn_w13': nrm(ks[19], (N_DENSE, D, 2 * F), jnp.float32) * D ** -0.5,
        'ffn_w2': nrm(ks[20], (N_DENSE, F, D), jnp.float32) * (DN_BETA * F ** -0.5),
        'moe_w_router': nrm(ks[21], (N_MOE, D, E), jnp.float32) * D ** -0.5,
        'moe_b_router': 0.01 * nrm(ks[22], (N_MOE, E), jnp.float32),
        'moe_w13': nrm(ks[23], (N_MOE, E, D, 2 * F), jnp.float32) * D ** -0.5,
        'moe_w2': nrm(jax.random.fold_in(ks[23], 1), (N_MOE, E, F, D), jnp.float32) * (DN_BETA * F ** -0.5),
    }


def reference(x, c, mod_w, mod_b, ln_g, ln_b, gm_w_in, gm_ln_g, gm_ln_b, gm_w_s, gm_b_s, gm_w_out,
              fox_w_in, fox_b_f, fox_w_out, pool_w_in, pool_w_grp, pool_scale, pool_w_out,
              ffn_w13, ffn_w2, moe_w_router, moe_b_router, moe_w13, moe_w2):
    mod = jnp.einsum('bd,ldm->lbm', jax.nn.silu(c), mod_w) + mod_b[:, None, :]
    for i in range(DEPTH):
        sh1, sc1, g1, sh2, sc2, g2 = jnp.split(mod[i][:, None, :], 6, axis=-1)
        h = x * (1 + sc1) + sh1
        kind, j = i % N_MIXERS, i // N_MIXERS
        if kind == 0:
            y = gmlp_chunk_mixer(h, gm_w_in[j], gm_ln_g[j], gm_ln_b[j], gm_w_s[j], gm_b_s[j], gm_w_out[j])
        elif kind == 1:
            y = forgetting_attention(h, fox_w_in[j], fox_b_f[j], fox_w_out[j])
        else:
            y = multiscale_pool_mixer(h, pool_w_in[j], pool_w_grp[j], pool_scale[j], pool_w_out[j])
        x = layer_norm(DN_ALPHA * x + (1 + g1) * y, ln_g[i, 0], ln_b[i, 0])
        h = x * (1 + sc2) + sh2
        if i % 2 == 0:
            y = swiglu(h, ffn_w13[i // 2], ffn_w2[i // 2])
        else:
            y = moe_swiglu(h, moe_w_router[i // 2], moe_b_router[i // 2], moe_w13[i // 2], moe_w2[i // 2])
        x = layer_norm(DN_ALPHA * x + (1 + g2) * y, ln_g[i, 1], ln_b[i, 1])
    return x
```

```python
import numpy as np
import concourse.bass as bass
import concourse.mybir as mybir
from concourse.bass_utils import run_bass_kernel_spmd

F32 = mybir.dt.float32
BF16 = mybir.dt.bfloat16
AF = mybir.ActivationFunctionType
OP = mybir.AluOpType
NCORES = 8
CHW = 2048
CH_ROWS = 256
CH_ELEMS = NCORES * CH_ROWS * CHW * 2
PIECE = CH_ROWS * CHW * 2
BIG = 30000.0


class Cfg:
    def __init__(self, D=1024, SEQ=8192, F=3584, TT=512, TTG=256, E=8, HD=64, GMG=8, PG=4, DEPTH=4, stop=None):
        self.D, self.SEQ, self.F, self.TT, self.TTG, self.E, self.HD = D, SEQ, F, TT, TTG, E, HD
        self.GMG, self.PG, self.DEPTH = GMG, PG, DEPTH
        self.B = 2
        self.L = SEQ // NCORES
        self.T = 2 * self.L
        self.KC = D // 128
        self.FC = F // 128
        self.H = D // HD
        self.GMW = 2 * D
        self.GC = self.GMW // 128
        self.alpha = (2 * DEPTH) ** 0.25
        self.stop = stop
        self.n_gm = (DEPTH + 2) // 3
        self.n_fox = (DEPTH + 1) // 3
        self.n_pool = DEPTH // 3
        self.n_dense = (DEPTH + 1) // 2
        self.n_moe = DEPTH // 2


class Packer:
    def __init__(self):
        self.parts = []
        self.pos = 0
        self.index = {}

    def add(self, name, blocks):
        nblk, p, free = blocks.shape
        assert p == 128
        bsz = 128 * free
        offs = []
        for k in range(nblk):
            if (self.pos % CH_ELEMS) + bsz > CH_ELEMS:
                pad = CH_ELEMS - (self.pos % CH_ELEMS)
                self.parts.append(np.zeros(pad, np.float32))
                self.pos += pad
            offs.append(self.pos)
            self.parts.append(np.ascontiguousarray(blocks[k]).reshape(-1))
            self.pos += bsz
        self.index[name] = (offs, free)

    def finish(self):
        pad = (-self.pos) % CH_ELEMS
        if pad:
            self.parts.append(np.zeros(pad, np.float32))
            self.pos += pad
        flat = np.concatenate(self.parts)
        nch = self.pos // CH_ELEMS
        return flat, nch


def stat_blocks(w, KCn):
    K, M = w.shape
    a = w.reshape(KCn, 128, M // 128, 128)
    return np.ascontiguousarray(a.transpose(2, 1, 0, 3)).reshape(M // 128, 128, KCn * 128)


def mov_blocks(w, KCn, ncol):
    K, M = w.shape
    a = w.reshape(KCn, 128, M // ncol, ncol)
    return np.ascontiguousarray(a.transpose(2, 1, 0, 3)).reshape(M // ncol, 128, KCn * ncol)


def pack_weights(cfg, inp):
    pk = Packer()
    D, F, KC, FC = cfg.D, cfg.F, cfg.KC, cfg.FC

    def add_ffn(tag, w13, w2):
        a = stat_blocks(w13[:, :F], KC).reshape(FC, 128, 1, KC * 128)
        b = stat_blocks(w13[:, F:], KC).reshape(FC, 128, 1, KC * 128)
        pk.add(tag + "w13", np.concatenate([a, b], axis=2).reshape(FC, 128, 2 * KC * 128))
        pk.add(tag + "w2", stat_blocks(w2, FC))

    def add_gm(j):
        w_in = inp["gm_w_in"][j]
        pk.add(f"gm{j}wu", stat_blocks(w_in[:, :cfg.GMW], KC))
        pk.add(f"gm{j}wv", mov_blocks(w_in[:, cfg.GMW:], KC, 512))
        ws = inp["gm_w_s"][j]
        wst = np.ascontiguousarray(ws.transpose(2, 0, 1)).reshape(1, 128, cfg.GMG * 128)
        pk.add(f"gm{j}ws", wst)
        pk.add(f"gm{j}wo", stat_blocks(inp["gm_w_out"][j], cfg.GC))

    def add_fox(j):
        w = inp["fox_w_in"][j]
        pk.add(f"fx{j}wq", stat_blocks(w[:, :D], KC))
        pk.add(f"fx{j}wk", stat_blocks(w[:, D:2 * D], KC))
        pk.add(f"fx{j}wv", mov_blocks(w[:, 2 * D:3 * D], KC, 512))
        wf = np.zeros((D, 128), np.float32)
        wf[:, :cfg.H] = w[:, 3 * D:]
        pk.add(f"fx{j}wf", stat_blocks(wf, KC))
        pk.add(f"fx{j}wo", stat_blocks(inp["fox_w_out"][j], KC))

    def add_pool(j):
        pk.add(f"pl{j}wi", stat_blocks(inp["pool_w_in"][j], KC))
        wg = inp["pool_w_grp"][j]
        gd = wg.shape[1]
        blks = [stat_blocks(wg[g], gd // 128) for g in range(cfg.PG)]
        pk.add(f"pl{j}wg", np.concatenate(blks, axis=0))
        pk.add(f"pl{j}wo", stat_blocks(inp["pool_w_out"][j], KC))

    for i in range(cfg.DEPTH):
        kind, j = i % 3, i // 3
        if kind == 0:
            add_gm(j)
        elif kind == 1:
            add_fox(j)
        else:
            add_pool(j)
        if i % 2 == 0:
            add_ffn(f"ff{i // 2}", inp["ffn_w13"][i // 2], inp["ffn_w2"][i // 2])
        else:
            m = i // 2
            wr = np.zeros((D, 128), np.float32)
            wr[:, :cfg.E] = inp["moe_w_router"][m]
            pk.add(f"mo{m}wr", mov_blocks(wr, KC, 128))
            for e in range(cfg.E):
                add_ffn(f"mo{m}e{e}", inp["moe_w13"][m, e], inp["moe_w2"][m, e])
    flat, nch = pk.finish()
    return flat, nch, pk.index


class ConstPack:
    BIGNAMES = ("gm_bs", "sel", "negc")

    def __init__(self):
        self.items = []

    def add(self, name, arr):
        arr = np.asarray(arr, np.float32).reshape(128, -1)
        self.items.append((name, arr))

    def finish(self):
        small = [(n, a) for n, a in self.items if not n.startswith(self.BIGNAMES)]
        big = [(n, a) for n, a in self.items if n.startswith(self.BIGNAMES)]
        index, cols, pos = {}, [], 0
        for n, a in small + big:
            index[n] = (pos, a.shape[1])
            cols.append(a)
            pos += a.shape[1]
        index["__nsmall__"] = (sum(a.shape[1] for _, a in small), 0)
        return np.concatenate(cols, axis=1), index


def fm(v, n):
    return np.ascontiguousarray(np.asarray(v, np.float32).reshape(n, 128).T)


def pack_consts(cfg, inp, core):
    cp = ConstPack()
    KC = cfg.KC
    cp.add("ln_g", np.stack([fm(inp["ln_g"][l, s], KC) for l in range(cfg.DEPTH) for s in range(2)], 1))
    cp.add("ln_b", np.stack([fm(inp["ln_b"][l, s], KC) for l in range(cfg.DEPTH) for s in range(2)], 1))
    ncol = 6 * cfg.D // NCORES
    nq = ncol // 128
    mb = np.stack([fm(inp["mod_b"][l, core * ncol:(core + 1) * ncol], nq) for l in range(cfg.DEPTH)], 1)
    cp.add("mod_b", mb)
    cp.add("cT", np.stack([fm(inp["c"][b], KC) for b in range(2)], 2))
    cp.add("gm_ln_g", np.stack([fm(inp["gm_ln_g"][j], cfg.GC) for j in range(cfg.n_gm)], 1))
    cp.add("gm_ln_b", np.stack([fm(inp["gm_ln_b"][j], cfg.GC) for j in range(cfg.n_gm)], 1))
    cp.add("gm_bs", np.broadcast_to(inp["gm_b_s"].reshape(1, -1), (128, inp["gm_b_s"].size)))
    tri = (np.arange(128)[:, None] <= np.arange(128)[None, :]).astype(np.float32)
    cp.add("tri", tri)
    cp.add("ident", np.eye(128, dtype=np.float32))
    cp.add("one_col", np.ones((128, 1), np.float32))
    cp.add("eps_col", np.full((128, 1), 1e-5, np.float32))
    sw = np.zeros((128, 128), np.float32)
    for kk in range(128):
        sw[kk, (kk + 64) % 128] = 1.0
    cp.add("swap", sw)
    if cfg.n_pool:
        cp.add("pool_scale", np.stack([fm(inp["pool_scale"][j], KC) for j in range(cfg.n_pool)], 1))
        rc = np.zeros((cfg.PG, 16), np.float32)
        for g in range(cfg.PG):
            win = 2 ** (g + 1)
            pos = np.arange(16) + core * cfg.L
            rc[g] = 1.0 / np.minimum(pos + 1, win)
        cp.add("pool_rc", np.broadcast_to(rc.reshape(1, -1), (128, rc.size)))
        prev = np.zeros(NCORES, np.float32)
        if core > 0:
            prev[core - 1] = 1.0
        cp.add("prev_onehot", np.broadcast_to(prev[None], (128, NCORES)))
        cp.add("not_first", np.full((128, 1), float(core > 0), np.float32))
    if cfg.n_moe:
        cp.add("moe_br", np.broadcast_to(inp["moe_b_router"].reshape(1, -1), (128, inp["moe_b_router"].size)))
        sel = np.zeros((128, cfg.E, 128), np.float32)
        for e in range(cfg.E):
            sel[e, e, :] = 1.0
        cp.add("sel", sel)
    if cfg.n_fox:
        bf = np.zeros((128, cfg.n_fox), np.float32)
        bf[:cfg.H, :] = inp["fox_b_f"].T
        cp.add("fox_bf", bf)
        r = np.arange(NCORES)
        cp.add("m_lt", np.broadcast_to((r < core).astype(np.float32)[None], (128, NCORES)))
        cp.add("m_gtbig", np.broadcast_to(((r > core) * BIG).astype(np.float32)[None], (128, NCORES)))
        cp.add("m_eq", np.broadcast_to((r == core).astype(np.float32)[None], (128, NCORES)))
        cp.add("negbig_eq", np.broadcast_to((-BIG * (r == core)).astype(np.float32)[None], (128, NCORES)))
        cp.add("m_neq", np.broadcast_to((r != core).astype(np.float32)[None], (128, NCORES)))
        sh = np.zeros((128, 64), np.float32)
        for m in range(64):
            sh[64 + m, m] = 1.0
        cp.add("shift", sh)
        for o in range(0, cfg.TT, 128):
            pat = ((o + np.arange(128))[:, None] <= np.arange(cfg.TT)[None, :]).astype(np.float32)
            cp.add(f"negc{o}", -BIG * (1.0 - pat))
    return cp.finish()


class Prog:
    ENG = ["pe", "act", "dve", "pool", "sp"]

    def __init__(self, nc):
        self.nc = nc
        self.streams = {e: [] for e in self.ENG}
        self.res_w = {}
        self.res_r = {}
        self.sem_count = {}
        self.waited = {e: {} for e in self.ENG}
        self.ps_i = 0

    def _deps(self, eng, reads, writes, is_pe_mm):
        deps = {}

        def need(sv, src_eng):
            if sv is None:
                return
            sem, val = sv
            if is_pe_mm and src_eng == "pe" and sem[0] == "eng" and sem[1] == "pe":
                return
            if val > deps.get(sem, 0):
                deps[sem] = val
        for r in reads:
            w = self.res_w.get(r)
            if w:
                need(w[0], w[1])
        for w_ in writes:
            w = self.res_w.get(w_)
            if w:
                need(w[0], w[1])
            for rr in self.res_r.get(w_, []):
                need(rr[0], rr[1])
        out = []
        for sem, val in deps.items():
            if self.waited[eng].get(sem, 0) < val:
                self.waited[eng][sem] = val
                out.append((sem, val))
        return out

    def op(self, eng, fn, reads=(), writes=(), kind="c", semkey=None):
        is_pe = (eng == "pe" and kind == "c")
        waits = self._deps(eng, reads, writes, is_pe)
        if kind == "c":
            self.eng_ops = getattr(self, "eng_ops", {})
            n_ = self.eng_ops.get(eng, 0)
            self.eng_ops[eng] = n_ + 1
            sem = ("eng", eng, n_ // 20000)
            inc = 1
        elif kind == "d":
            sem = ("dma", semkey if semkey is not None else writes[0])
            inc = 16
        else:
            sem = ("cc", 0)
            inc = 1
        val = self.sem_count.get(sem, 0) + inc
        self.sem_count[sem] = val
        self.streams[eng].append((waits, fn, sem, inc))
        for r in reads:
            self.res_r.setdefault(r, []).append(((sem, val), eng))
        for w_ in writes:
            self.res_w[w_] = ((sem, val), eng)
            self.res_r[w_] = []
        return (sem, val)

    def barrier(self):
        for eng in self.ENG:
            waits = []
            for sem, cnt in self.sem_count.items():
                if self.waited[eng].get(sem, 0) < cnt:
                    self.waited[eng][sem] = cnt
                    waits.append((sem, cnt))
            if waits:
                self.streams[eng].append((waits, None, None, 0))
        self.res_w = {}
        self.res_r = {}

    def final_wait(self, eng, sv):
        self.streams[eng].append(([sv], None, None, 0))

    def emit(self, stack):
        nc = self.nc
        sems = {}
        for key in self.sem_count:
            sems[key] = stack.enter_context(nc.semaphore("s%d" % len(sems)))
        block = stack.enter_context(nc.Block())
        engs = {"pe": block.tensor, "act": block.scalar, "dve": block.vector, "pool": block.gpsimd, "sp": block.sync}

        def mk(ename):
            stream = self.streams[ename]

            def body(eng):
                for waits, fn, sem, inc in stream:
                    for (s, v) in waits:
                        eng.wait_ge(sems[s], v)
                    if fn is not None:
                        ins = fn(eng)
                        ins.then_inc(sems[sem], inc)
            return body
        for ename in self.ENG:
            engs[ename](mk(ename))


def build_program(cfg, nch, widx, cidx, ncst):
    nc = bass.Bass("TRN2", target_bir_lowering=False)
    D, KC, FC, TT, T, L, E = cfg.D, cfg.KC, cfg.FC, cfg.TT, cfg.T, cfg.L, cfg.E
    NT = T // TT
    ncol = 6 * D // NCORES
    nq = ncol // 128
    xT = nc.dram_tensor("xT", [D, T], F32, kind="ExternalInput").ap()
    wsh = nc.dram_tensor("wsh", [nch * 2 * CH_ROWS, CHW], F32, kind="ExternalInput").ap()
    modw = nc.dram_tensor("modw", [cfg.DEPTH * nq * 128, KC * 128], F32, kind="ExternalInput").ap()
    cst_d = nc.dram_tensor("cst", [128, ncst], F32, kind="ExternalInput").ap()
    outT = nc.dram_tensor("outT", [D, T], F32, kind="ExternalOutput").ap()
    wloc = [nc.dram_tensor("wloc%d" % i, [CH_ROWS, CHW], F32).ap() for i in range(2)]
    walls = [nc.dram_tensor("wall%d" % i, [NCORES * CH_ROWS, CHW], F32).ap() for i in range(nch)]
    walls_bf = [w_.bitcast(BF16).rearrange("a b -> (a b)") for w_ in walls]
    modloc = nc.dram_tensor("modloc", [128, cfg.DEPTH * nq * 2], F32).ap()
    modall = nc.dram_tensor("modall", [NCORES * 128, cfg.DEPTH * nq * 2], F32).ap()
    groups = [list(range(NCORES))]

    import contextlib
    with contextlib.ExitStack() as stack:
        P = Prog(nc)

        def sb(name, shape, dt):
            return stack.enter_context(nc.sbuf_tensor(name, shape, dt))

        def C(name):
            o, n = cidx[name]
            assert o + n <= cidx["__nsmall__"][0], name
            return cst[:, o:o + n]

        def load_big(name, dst, key):
            o, n = cidx[name]
            P.op("sp", lambda e: e.dma_start(out=dst, in_=cst_d[:, o:o + n]), writes=[key], kind="d")

        ARENA = 78 * 1024
        arena = sb("arena", [128, ARENA // 2], BF16)
        apos = [0]

        def areset():
            P.barrier()
            apos[0] = 0

        def ar(name, shape, dt):
            n = 1
            for d in shape[1:]:
                n *= d
            nb = n * (4 if dt == F32 else 2)
            nb = (nb + 31) // 32 * 32
            off = apos[0]
            assert off + nb <= ARENA, ("arena overflow", name, off, nb)
            apos[0] = off + nb
            v = arena[:, off // 2:(off + nb) // 2]
            if dt == F32:
                v = v.bitcast(F32)
            v = v[:, :n]
            if len(shape) == 3:
                v = v.rearrange("p (a b) -> p a b", a=shape[1])
            elif len(shape) == 4:
                v = v.rearrange("p (a b c) -> p a b c", a=shape[1], b=shape[2])
            return v

        nsmall = cidx["__nsmall__"][0]
        cst = sb("cst_sb", [128, nsmall], F32)
        S = sb("S", [128, KC, T], F32)
        modsb = sb("modsb", [128, NCORES, cfg.DEPTH, nq, 2], F32)
        ones_f = sb("ones_f", [128, 128], F32)
        ones_b = sb("ones_b", [128, 128], BF16)
        ident_b = sb("ident_b", [128, 128], BF16)
        psum = [stack.enter_context(nc.psum_tensor("ps%d" % i, [128, 512], F32)) for i in range(8)]

        ps_reserved = set()

        def next_ps():
            while True:
                i = P.ps_i
                P.ps_i = (i + 1) % 8
                if i not in ps_reserved:
                    return i

        P.op("sp", lambda e: e.dma_start(out=cst[:], in_=cst_d[:, 0:nsmall]), writes=["cst"], kind="d")
        P.op("pool", lambda e: e.memset(ones_f[:], 1.0 / D), writes=["ones_f"])
        P.op("pool", lambda e: e.memset(ones_b[:], 1.0), writes=["ones_b"])
        P.op("dve", lambda e: e.tensor_copy(out=ident_b[:], in_=C("ident")), reads=["cst"], writes=["ident_b"])

        apos[0] = 0
        stg = [ar("stg%d" % i, [128, CHW], F32) for i in range(3)]
        stb = [ar("stb%d" % i, [128, CHW], BF16) for i in range(3)]
        cast_engs = ["dve", "act", "pool"]
        k = 0
        for j in range(nch):
            wl = wloc[j % 2]
            wl_bf = wl.bitcast(BF16).rearrange("a (t b) -> (a t) b", t=2)
            for q in range(4):
                s = k % 3
                row0 = j * 2 * CH_ROWS + q * 128
                P.op("sp", lambda e, s=s, row0=row0: e.dma_start(out=stg[s][:], in_=wsh[row0:row0 + 128, :]),
                     writes=["stg%d" % s], kind="d")
                ce = cast_engs[k % 3]
                if ce == "act":
                    P.op("act", lambda e, s=s: e.activation(out=stb[s][:], in_=stg[s][:], func=AF.Copy),
                         reads=["stg%d" % s], writes=["stb%d" % s])
                else:
                    P.op(ce, lambda e, s=s: e.tensor_copy(out=stb[s][:], in_=stg[s][:]),
                         reads=["stg%d" % s], writes=["stb%d" % s])
                P.op("act", lambda e, s=s, q=q, wl_bf=wl_bf: e.dma_start(out=wl_bf[q * 128:(q + 1) * 128, :], in_=stb[s][:]),
                     reads=["stb%d" % s], writes=["wloc%d" % (j % 2)], kind="d", semkey="wloc%d_%d" % (j % 2, q))
                k += 1
            P.op("pool", lambda e, wl=wl, j=j: e.collective_compute(
                "AllGather", OP.bypass, replica_groups=groups, ins=[wl], outs=[walls[j]]),
                reads=["wloc%d" % (j % 2)], writes=["wall%d" % j], kind="cc")

        def wres(off):
            return "wall%d" % (off // CH_ELEMS)

        def wblock(name, kblk):
            offs, free = widx[name]
            off = offs[kblk]
            cj, o2 = off // CH_ELEMS, off % CH_ELEMS
            return walls_bf[cj][o2:o2 + 128 * free].rearrange("(p f) -> p f", p=128), wres(off), free

        silc = sb("silc", [128, KC, 2], F32)
        P.op("act", lambda e: e.activation(out=silc[:].rearrange("p a b -> p (a b)"), in_=C("cT"), func=AF.Silu),
             reads=["cst"], writes=["silc"])
        mw = [ar("mw%d" % i, [128, KC * 128], F32) for i in range(2)]
        mloc = sb("mloc", [128, cfg.DEPTH, nq, 2], F32)
        mbo, _ = cidx["mod_b"]
        for l in range(cfg.DEPTH):
            for qi in range(nq):
                idx = l * nq + qi
                s = idx % 2
                P.op("sp", lambda e, s=s, idx=idx: e.dma_start(out=mw[s][:], in_=modw[idx * 128:(idx + 1) * 128, :]),
                     writes=["mw%d" % s], kind="d")
                pi = next_ps()
                for kc in range(KC):
                    P.op("pe", lambda e, pi=pi, s=s, kc=kc: e.matmul(psum[pi][:, 0:2], mw[s][:, kc * 128:(kc + 1) * 128],
                                                                      silc[:, kc, :], start=(kc == 0), stop=(kc == KC - 1)),
                         reads=["mw%d" % s, "silc"], writes=["ps%d" % pi])
                bcol = cst[:, mbo + l * nq + qi: mbo + l * nq + qi + 1]
                P.op("dve", lambda e, pi=pi, l=l, qi=qi, bcol=bcol: e.tensor_scalar(
                    out=mloc[:, l, qi, :], in0=psum[pi][:, 0:2], scalar1=bcol, scalar2=None, op0=OP.add),
                    reads=["ps%d" % pi, "cst"], writes=["mloc"])
        P.op("act", lambda e: e.dma_start(out=modloc, in_=mloc[:].rearrange("p a b c -> p (a b c)")),
             reads=["mloc"], writes=["modloc"], kind="d")
        P.op("pool", lambda e: e.collective_compute("AllGather", OP.bypass, replica_groups=groups, ins=[modloc], outs=[modall]),
             reads=["modloc"], writes=["modall"], kind="cc")
        P.op("sp", lambda e: e.dma_start(out=modsb[:].rearrange("p r a b c -> p r (a b c)"),
                                         in_=modall.rearrange("(r p) f -> p r f", p=128)),
             reads=["modall"], writes=["modsb"], kind="d")

        def modcol(l, jcomp, kc, b):
            qg = jcomp * KC + kc
            return modsb[:, qg // nq, l, qg % nq, b:b + 1]

        dsc = sb("dsc", [128, cfg.DEPTH, 2, 2, KC, 2], F32)
        for l in range(cfg.DEPTH):
            for sub in range(2):
                for kc in range(KC):
                    P.op("pool", lambda e, l=l, sub=sub, kc=kc: e.tensor_scalar(
                        out=dsc[:, l, sub, 0, kc, :], in0=modsb[:, (((3 * sub + 1) * KC + kc) // nq), l, ((3 * sub + 1) * KC + kc) % nq, :],
                        scalar1=1.0, scalar2=1.0 / cfg.alpha, op0=OP.add, op1=OP.mult), reads=["modsb"], writes=["dsc"])
                    P.op("pool", lambda e, l=l, sub=sub, kc=kc: e.tensor_scalar(
                        out=dsc[:, l, sub, 1, kc, :], in0=modsb[:, (((3 * sub + 2) * KC + kc) // nq), l, ((3 * sub + 2) * KC + kc) % nq, :],
                        scalar1=1.0, scalar2=None, op0=OP.add), reads=["modsb"], writes=["dsc"])
        lng = sb("lng", [128, cfg.DEPTH * 2 * KC], F32)
        lnb = sb("lnb", [128, cfg.DEPTH * 2 * KC], F32)
        P.op("pool", lambda e: e.tensor_scalar(out=lng[:], in0=C("ln_g"), scalar1=cfg.alpha, scalar2=None, op0=OP.mult),
             reads=["cst"], writes=["lng"])
        P.op("pool", lambda e: e.tensor_scalar(out=lnb[:], in0=C("ln_b"), scalar1=cfg.alpha, scalar2=None, op0=OP.mult),
             reads=["cst"], writes=["lnb"])

        for kc in range(KC):
            P.op("sp", lambda e, kc=kc: e.dma_start(out=S[:, kc, :], in_=xT[kc * 128:(kc + 1) * 128, :]),
                 writes=[("S", kc, t) for t in range(NT)], kind="d", semkey="Sld%d" % kc)
        for kc in range(KC):
            for t in range(NT):
                P.op("pool", lambda e, kc=kc, t=t: e.tensor_scalar(out=S[:, kc, t * TT:(t + 1) * TT], in0=S[:, kc, t * TT:(t + 1) * TT],
                                                                    scalar1=cfg.alpha, scalar2=None, op0=OP.mult),
                     reads=[("S", kc, t)], writes=[("S", kc, t)])

        hbuf = [sb("h%d" % i, [128, KC, TT], BF16) for i in range(2)]
        rbuf = sb("rbuf", [128, KC, TT], F32)
        ybuf = sb("ybuf", [128, KC, TT], F32)
        sqb = ybuf
        stat = sb("stat", [128, 3, TT], F32)
        hcnt = [0]

        def tile_batch(t):
            return (t * TT) // L

        def make_h(l, sub, t, tt=None, tsz=None):
            hi = hcnt[0] % 2
            hcnt[0] += 1
            b = tile_batch(t)
            for kc in range(KC):
                eng = "act" if kc % 2 == 0 else "dve"
                shcol = modcol(l, 3 * sub, kc, b)
                m1 = dsc[:, l, sub, 0, kc, b:b + 1]
                if eng == "act":
                    P.op("act", lambda e, hi=hi, kc=kc, t=t, m1=m1, shcol=shcol: e.activation(
                        out=hbuf[hi][:, kc, :], in_=S[:, kc, t * TT:(t + 1) * TT], func=AF.Identity, scale=m1, bias=shcol),
                        reads=[("S", kc, t), "dsc", "modsb"], writes=[("h", hi, kc)])
                else:
                    P.op("dve", lambda e, hi=hi, kc=kc, t=t, m1=m1, shcol=shcol: e.tensor_scalar(
                        out=hbuf[hi][:, kc, :], in0=S[:, kc, t * TT:(t + 1) * TT], scalar1=m1, scalar2=shcol, op0=OP.mult, op1=OP.add),
                        reads=[("S", kc, t), "dsc", "modsb"], writes=[("h", hi, kc)])
            return hi

        def residual_ln(l, sub, t, ysrc):
            b = tile_batch(t)
            for kc in range(KC):
                yap, yres = ysrc(kc)
                gcol = dsc[:, l, sub, 1, kc, b:b + 1]
                P.op("dve", lambda e, kc=kc, yap=yap, gcol=gcol, t=t: e.scalar_tensor_tensor(
                    out=rbuf[:, kc, :], in0=yap, scalar=gcol, in1=S[:, kc, t * TT:(t + 1) * TT], op0=OP.mult, op1=OP.add),
                    reads=[yres, ("S", kc, t), "dsc"], writes=[("r", kc)])
                P.op("act", lambda e, kc=kc: e.activation(out=sqb[:, kc, :], in_=rbuf[:, kc, :], func=AF.Square),
                     reads=[("r", kc)], writes=[("y", kc)])
            p1, p2 = next_ps(), next_ps()
            for kc in range(KC):
                P.op("pe", lambda e, kc=kc, p1=p1: e.matmul(psum[p1][:, :TT], ones_f[:], rbuf[:, kc, :], start=(kc == 0), stop=(kc == KC - 1)),
                     reads=[("r", kc), "ones_f"], writes=["ps%d" % p1])
            for kc in range(KC):
                P.op("pe", lambda e, kc=kc, p2=p2: e.matmul(psum[p2][:, :TT], ones_f[:], sqb[:, kc, :], start=(kc == 0), stop=(kc == KC - 1)),
                     reads=[("y", kc), "ones_f"], writes=["ps%d" % p2])
            P.op("act", lambda e, p1=p1: e.activation(out=stat[:, 0, :], in_=psum[p1][:, :TT], func=AF.Copy), reads=["ps%d" % p1], writes=["stat0"])
            P.op("dve", lambda e: e.tensor_tensor(out=stat[:, 2, :], in0=stat[:, 0, :], in1=stat[:, 0, :], op=OP.mult), reads=["stat0"], writes=["stat2"])
            P.op("dve", lambda e, p2=p2: e.tensor_tensor(out=stat[:, 1, :], in0=psum[p2][:, :TT], in1=stat[:, 2, :], op=OP.subtract),
                 reads=["ps%d" % p2, "stat2"], writes=["stat1"])
            P.op("act", lambda e: e.activation(out=stat[:, 1, :], in_=stat[:, 1, :], func=AF.Ln, bias=C("eps_col")), reads=["stat1", "cst"], writes=["stat1"])
            P.op("act", lambda e: e.activation(out=stat[:, 1, :], in_=stat[:, 1, :], func=AF.Exp, scale=-0.5), reads=["stat1"], writes=["stat1"])
            li = (l * 2 + sub) * KC
            for kc in range(KC):
                P.op("dve", lambda e, kc=kc: e.tensor_tensor(out=rbuf[:, kc, :], in0=rbuf[:, kc, :], in1=stat[:, 0, :], op=OP.subtract),
                     reads=[("r", kc), "stat0"], writes=[("r", kc)])
                P.op("pool", lambda e, kc=kc: e.tensor_tensor(out=rbuf[:, kc, :], in0=rbuf[:, kc, :], in1=stat[:, 1, :], op=OP.mult),
                     reads=[("r", kc), "stat1"], writes=[("r", kc)])
                P.op("act", lambda e, kc=kc, t=t, li=li: e.activation(out=S[:, kc, t * TT:(t + 1) * TT], in_=rbuf[:, kc, :], func=AF.Identity,
                                                                      scale=lng[:, li + kc:li + kc + 1], bias=lnb[:, li + kc:li + kc + 1]),
                     reads=[("r", kc), "lng", "lnb"], writes=[("S", kc, t)])

        FB = {}

        def ffn_alloc():
            FB["w13r"] = [ar("w13r%d" % i, [128, 2 * KC * 128], BF16) for i in range(3)]
            FB["w2r"] = [ar("w2r%d" % i, [128, FC * 128], BF16) for i in range(2)]
            FB["Gt"] = ar("Gt", [128, FC, TT], BF16)
            FB["sil"] = [ar("sil%d" % i, [128, TT], F32) for i in range(2)]
        ring_cnt = {"w13": 0, "w2": 0, "sil": 0}

        def ffn_tile(tag, hi, yout):
            w13r, w2r, Gt, sil = FB["w13r"], FB["w2r"], FB["Gt"], FB["sil"]
            for fc in range(FC):
                s = ring_cnt["w13"] % 3
                ring_cnt["w13"] += 1
                src, res, free = wblock(tag + "w13", fc)
                P.op("sp", lambda e, s=s, src=src: e.dma_start(out=w13r[s][:], in_=src), reads=[res], writes=["w13r%d" % s], kind="d")
                pa, pb = next_ps(), next_ps()
                for half, pi in ((0, pa), (1, pb)):
                    for kc in range(KC):
                        o = (half * KC + kc) * 128
                        P.op("pe", lambda e, s=s, o=o, pi=pi, kc=kc: e.matmul(psum[pi][:, :TT], w13r[s][:, o:o + 128], hbuf[hi][:, kc, :],
                                                                              start=(kc == 0), stop=(kc == KC - 1)),
                             reads=["w13r%d" % s, ("h", hi, kc)], writes=["ps%d" % pi])
                si = ring_cnt["sil"] % 2
                ring_cnt["sil"] += 1
                P.op("act", lambda e, si=si, pa=pa: e.activation(out=sil[si][:], in_=psum[pa][:, :TT], func=AF.Silu),
                     reads=["ps%d" % pa], writes=["sil%d" % si])
                P.op("dve", lambda e, si=si, pb=pb, fc=fc: e.tensor_tensor(out=Gt[:, fc, :], in0=sil[si][:], in1=psum[pb][:, :TT], op=OP.mult),
                     reads=["sil%d" % si, "ps%d" % pb], writes=[("G", fc)])
            for dc in range(KC):
                s = ring_cnt["w2"] % 2
                ring_cnt["w2"] += 1
                src, res, free = wblock(tag + "w2", dc)
                P.op("sp", lambda e, s=s, src=src: e.dma_start(out=w2r[s][:], in_=src), reads=[res], writes=["w2r%d" % s], kind="d")
                py = next_ps()
                for fc in range(FC):
                    P.op("pe", lambda e, s=s, fc=fc, py=py: e.matmul(psum[py][:, :TT], w2r[s][:, fc * 128:(fc + 1) * 128], Gt[:, fc, :],
                                                                     start=(fc == 0), stop=(fc == FC - 1)),
                         reads=["w2r%d" % s, ("G", fc)], writes=["ps%d" % py])
                yout(dc, py)


        def dense_ffn_layer(l, i):
            areset()
            ffn_alloc()
            for t in range(NT):
                hi = make_h(l, 1, t)

                def yout(dc, py):
                    P.op("act", lambda e, dc=dc, py=py: e.activation(out=ybuf[:, dc, :], in_=psum[py][:, :TT], func=AF.Copy),
                         reads=["ps%d" % py], writes=[("y", dc)])
                ffn_tile("ff%d" % i, hi, yout)
                residual_ln(l, 1, t, lambda kc: (ybuf[:, kc, :], ("y", kc)))

        def moe_layer(l, m):
            areset()
            ffn_alloc()
            gwT = ar("gwT%d" % m, [128, TT], F32)
            lg = ar("lg%d" % m, [128, 128], F32)
            gw = ar("gw%d" % m, [128, 128], F32)
            mx = ar("mx%d" % m, [128, 8], F32)
            tmp1 = ar("mtmp%d" % m, [128, 8], F32)
            gwb = ar("gwb%d" % m, [128, TT], F32)
            wrs = ar("wrs%d" % m, [128, KC * 128], BF16)
            selt = ar("selt%d" % m, [128, E * 128], F32)
            load_big("sel", selt[:], "selt")
            ident_f = C("ident")
            bro, _ = cidx["moe_br"]
            src, res, free = wblock("mo%dwr" % m, 0)
            P.op("sp", lambda e, src=src: e.dma_start(out=wrs[:], in_=src), reads=[res], writes=["wrs"], kind="d")
            for t in range(NT):
                hi = make_h(l, 1, t)
                for tb in range(TT // 128):
                    pl = next_ps()
                    for kc in range(KC):
                        P.op("pe", lambda e, kc=kc, tb=tb, pl=pl, hi=hi: e.matmul(psum[pl][:, :128], hbuf[hi][:, kc, tb * 128:(tb + 1) * 128],
                                                                            wrs[:, kc * 128:(kc + 1) * 128], start=(kc == 0), stop=(kc == KC - 1)),
                             reads=[("h", hi, kc), "wrs"], writes=["ps%d" % pl])
                    P.op("dve", lambda e, pl=pl: e.tensor_tensor(out=lg[:, :E], in0=psum[pl][:, :E], in1=cst[:, bro + m * E: bro + (m + 1) * E], op=OP.add),
                         reads=["ps%d" % pl, "cst"], writes=["lg"])
                    P.op("dve", lambda e: e.max(out=mx[:], in_=lg[:, :E]), reads=["lg"], writes=["mx"])
                    P.op("dve", lambda e: e.tensor_scalar(out=gw[:, :E], in0=lg[:, :E], scalar1=mx[:, 0:1], scalar2=None, op0=OP.subtract),
                         reads=["lg", "mx"], writes=["gw"])
                    P.op("act", lambda e: e.activation(out=gw[:, :E], in_=gw[:, :E], func=AF.Exp), reads=["gw"], writes=["gw"])
                    P.op("dve", lambda e: e.tensor_tensor(out=tmp1[:, 0:1], in0=mx[:, 1:2], in1=mx[:, 0:1], op=OP.subtract), reads=["mx"], writes=["tmp1"])
                    P.op("act", lambda e: e.activation(out=tmp1[:, 1:2], in_=tmp1[:, 0:1], func=AF.Exp), reads=["tmp1"], writes=["tmp1b"])
                    P.op("dve", lambda e: e.tensor_scalar(out=tmp1[:, 2:3], in0=tmp1[:, 1:2], scalar1=1.0, scalar2=None, op0=OP.add), reads=["tmp1b"], writes=["tmp1c"])
                    P.op("dve", lambda e: e.reciprocal(out=tmp1[:, 3:4], in_=tmp1[:, 2:3]), reads=["tmp1c"], writes=["tmp1d"])
                    P.op("dve", lambda e: e.tensor_scalar(out=lg[:, :E], in0=lg[:, :E], scalar1=mx[:, 1:2], scalar2=tmp1[:, 3:4], op0=OP.is_ge, op1=OP.mult),
                         reads=["lg", "mx", "tmp1d"], writes=["lg"])
                    P.op("dve", lambda e: e.tensor_tensor(out=gw[:, :E], in0=gw[:, :E], in1=lg[:, :E], op=OP.mult), reads=["gw", "lg"], writes=["gw"])
                    pt = next_ps()
                    P.op("pe", lambda e, pt=pt: e.transpose(psum[pt][:E, :128], gw[:, :E], ident_f), reads=["gw", "cst"], writes=["ps%d" % pt])
                    P.op("act", lambda e, pt=pt, tb=tb: e.activation(out=gwT[:E, tb * 128:(tb + 1) * 128], in_=psum[pt][:E, :128], func=AF.Copy),
                         reads=["ps%d" % pt], writes=["gwT"])
                first = [True] * KC
                for ex in range(E):
                    pg = next_ps()
                    P.op("pe", lambda e, ex=ex, pg=pg: e.matmul(psum[pg][:, :TT], selt[:E, ex * 128:(ex + 1) * 128], gwT[:E, :],
                                                                 start=True, stop=True), reads=["gwT", "selt"], writes=["ps%d" % pg])
                    P.op("act", lambda e, pg=pg: e.activation(out=gwb[:], in_=psum[pg][:, :TT], func=AF.Copy), reads=["ps%d" % pg], writes=["gwb"])

                    def yout(dc, py, ex=ex):
                        if ex == 0:
                            P.op("dve", lambda e, dc=dc, py=py: e.tensor_tensor(out=ybuf[:, dc, :], in0=psum[py][:, :TT], in1=gwb[:], op=OP.mult),
                                 reads=["ps%d" % py, "gwb"], writes=[("y", dc)])
                        else:
                            P.op("dve", lambda e, dc=dc, py=py: e.tensor_tensor(out=rbuf[:, dc, :], in0=psum[py][:, :TT], in1=gwb[:], op=OP.mult),
                                 reads=["ps%d" % py, "gwb"], writes=[("r", dc)])
                            P.op("pool", lambda e, dc=dc: e.tensor_tensor(out=ybuf[:, dc, :], in0=ybuf[:, dc, :], in1=rbuf[:, dc, :], op=OP.add),
                                 reads=[("r", dc), ("y", dc)], writes=[("y", dc)])
                    ffn_tile("mo%de%d" % (m, ex), hi, yout)
                residual_ln(l, 1, t, lambda kc: (ybuf[:, kc, :], ("y", kc)))

        wstr = []
        wstr_cnt = [0]

        def wstr_alloc():
            wstr.clear()
            wstr.extend([ar("wstr%d" % i, [128, max(KC, cfg.GC) * 128], BF16) for i in range(2)])

        def load_stat(name, kblk):
            s = wstr_cnt[0] % 2
            wstr_cnt[0] += 1
            src, res, free = wblock(name, kblk)
            P.op("sp", lambda e, dst=wstr[s], src=src, free=free: e.dma_start(out=dst[:, :free], in_=src), reads=[res], writes=["wstr%d" % s], kind="d")
            return s

        def proj_fm(name, mc, rhs_fn, nk, ntok, pi):
            s = load_stat(name, mc)
            for kk in range(nk):
                rap, rres = rhs_fn(kk)
                P.op("pe", lambda e, wt=wstr[s], kk=kk, rap=rap, pi=pi: e.matmul(psum[pi][:, :ntok], wt[:, kk * 128:(kk + 1) * 128], rap,
                                                                           start=(kk == 0), stop=(kk == nk - 1)),
                     reads=["wstr%d" % s, rres], writes=["ps%d" % pi])

        def gmlp_layer(l, j):
            TG = cfg.TTG
            GC = cfg.GC
            gd_chunks = GC // cfg.GMG
            areset()
            wstr_alloc()
            wsb = ar("gm_ws%d" % j, [128, cfg.GMG * 128], BF16)
            MB = ar("gm_MB%d" % j, [128, GC, 128], F32)
            wv = [ar("gm_wv%d_%d" % (j, i), [128, KC * 512], BF16) for i in range(1)]
            u = ar("gm_u%d" % j, [128, GC, TG], BF16)
            um = ar("gm_um%d" % j, [128, GC, TG], BF16)
            vpre = ar("gm_vpre%d" % j, [128, TG // 128, cfg.GMW], F32)
            vhat = [ar("gm_vhat%d_%d" % (j, i), [128, cfg.GMW], BF16) for i in range(2)]
            bst = ar("gm_bst%d" % j, [128, cfg.GMW // 512 + 1, 6], F32)
            mv = ar("gm_mv%d" % j, [128, 8], F32)
            mixed = ar("gm_mixed%d" % j, [128, 128], F32)
            bsT = ar("gm_bsT%d" % j, [128, cfg.GMG * 128], F32)
            ob_, nb_ = cidx["gm_bs"]
            P.op("sp", lambda e: e.dma_start(out=bsT[:], in_=cst_d[:, ob_ + j * cfg.GMG * 128: ob_ + (j + 1) * cfg.GMG * 128]), writes=["gm_bsT"], kind="d")
            trio, _ = cidx["tri"]
            glg, _ = cidx["gm_ln_g"]
            glb, _ = cidx["gm_ln_b"]
            src, res, free = wblock("gm%dws" % j, 0)
            P.op("sp", lambda e, src=src: e.dma_start(out=wsb[:], in_=src), reads=[res], writes=["gm_ws"], kind="d")
            for g in range(cfg.GMG):
                P.op("dve", lambda e, g=g: e.tensor_tensor(out=wsb[:, g * 128:(g + 1) * 128], in0=wsb[:, g * 128:(g + 1) * 128],
                                                            in1=cst[:, trio:trio + 128], op=OP.mult), reads=["gm_ws", "cst"], writes=["gm_ws"])
            for g in range(cfg.GMG):
                pr = next_ps()
                P.op("pe", lambda e, g=g, pr=pr: e.matmul(psum[pr][:, :128], ones_b[:], wsb[:, g * 128:(g + 1) * 128], start=True, stop=True),
                     reads=["gm_ws", "ones_b"], writes=["ps%d" % pr])
                for cc in range(gd_chunks):
                    fc = g * gd_chunks + cc
                    P.op("dve", lambda e, g=g, fc=fc, pr=pr: e.scalar_tensor_tensor(
                        out=MB[:, fc, :], in0=psum[pr][:, :128], scalar=cst[:, glb + j * GC + fc: glb + j * GC + fc + 1],
                        in1=bsT[:, g * 128:(g + 1) * 128], op0=OP.mult, op1=OP.add),
                        reads=["ps%d" % pr, "cst", "gm_bsT"], writes=["gm_MB"])
            NTG = T // TG
            for t in range(NTG):
                b = (t * TG) // L
                hi = hcnt[0] % 2
                hcnt[0] += 1
                for kc in range(KC):
                    shcol = modcol(l, 0, kc, b)
                    m1 = dsc[:, l, 0, 0, kc, b:b + 1]
                    P.op("act", lambda e, hi=hi, kc=kc, t=t, m1=m1, shcol=shcol: e.activation(
                        out=hbuf[hi][:, kc, :TG], in_=S[:, kc, t * TG:(t + 1) * TG], func=AF.Identity, scale=m1, bias=shcol),
                        reads=[("S", kc, (t * TG) // TT), "dsc", "modsb"], writes=[("h", hi, kc)])
                for mc in range(GC):
                    pu = next_ps()
                    proj_fm("gm%dwu" % j, mc, lambda kk: (hbuf[hi][:, kk, :TG], ("h", hi, kk)), KC, TG, pu)
                    P.op("act", lambda e, mc=mc, pu=pu: e.activation(out=u[:, mc, :], in_=psum[pu][:, :TG], func=AF.Gelu),
                         reads=["ps%d" % pu], writes=[("gm_u", mc)])
                for cg in range(cfg.GMW // 512):
                    s = 0
                    src, res, free = wblock("gm%dwv" % j, cg)
                    P.op("sp", lambda e, s=s, src=src: e.dma_start(out=wv[s][:], in_=src), reads=[res], writes=["gm_wv%d" % s], kind="d")
                    for tb in range(TG // 128):
                        pv = next_ps()
                        for kc in range(KC):
                            P.op("pe", lambda e, s=s, kc=kc, tb=tb, pv=pv, hi=hi: e.matmul(psum[pv][:, :512], hbuf[hi][:, kc, tb * 128:(tb + 1) * 128],
                                                                                    wv[s][:, kc * 512:(kc + 1) * 512], start=(kc == 0), stop=(kc == KC - 1)),
                                 reads=["gm_wv%d" % s, ("h", hi, kc)], writes=["ps%d" % pv])
                        P.op("act", lambda e, tb=tb, cg=cg, pv=pv: e.activation(out=vpre[:, tb, cg * 512:(cg + 1) * 512], in_=psum[pv][:, :512], func=AF.Gelu),
                             reads=["ps%d" % pv], writes=[("gm_vpre", tb)])
                for tb in range(TG // 128):
                    ncg = cfg.GMW // 512
                    for cg in range(ncg):
                        P.op("dve", lambda e, tb=tb, cg=cg: e.bn_stats(out=bst[:, cg, :], in_=vpre[:, tb, cg * 512:(cg + 1) * 512]),
                             reads=[("gm_vpre", tb)], writes=["gm_bst"])
                    P.op("dve", lambda e, ncg=ncg: e.bn_aggr(out=mv[:, 0:2], in_=bst[:, :ncg, :].rearrange("p a b -> p (a b)")),
                         reads=["gm_bst"], writes=["gm_mv"])
                    P.op("act", lambda e: e.activation(out=mv[:, 2:3], in_=mv[:, 1:2], func=AF.Ln, bias=C("eps_col")), reads=["gm_mv", "cst"], writes=["gm_mv2"])
                    P.op("act", lambda e: e.activation(out=mv[:, 2:3], in_=mv[:, 2:3], func=AF.Exp, scale=-0.5), reads=["gm_mv2"], writes=["gm_mv2"])
                    vh = tb % 2
                    P.op("dve", lambda e, tb=tb, vh=vh: e.tensor_scalar(out=vhat[vh][:], in0=vpre[:, tb, :], scalar1=mv[:, 0:1], scalar2=mv[:, 2:3],
                                                                          op0=OP.subtract, op1=OP.mult),
                         reads=[("gm_vpre", tb), "gm_mv", "gm_mv2"], writes=["gm_vhat%d" % vh])
                    for fc in range(GC):
                        g = fc // gd_chunks
                        pm = next_ps()
                        P.op("pe", lambda e, vh=vh, fc=fc, g=g, pm=pm: e.matmul(psum[pm][:, :128], vhat[vh][:, fc * 128:(fc + 1) * 128],
                                                                                 wsb[:, g * 128:(g + 1) * 128], start=True, stop=True),
                             reads=["gm_vhat%d" % vh, "gm_ws"], writes=["ps%d" % pm])
                        P.op("dve", lambda e, fc=fc, pm=pm: e.scalar_tensor_tensor(
                            out=mixed[:], in0=psum[pm][:, :128], scalar=cst[:, glg + j * GC + fc: glg + j * GC + fc + 1], in1=MB[:, fc, :],
                            op0=OP.mult, op1=OP.add), reads=["ps%d" % pm, "gm_MB", "cst"], writes=["gm_mixed"])
                        P.op("pool", lambda e, fc=fc, tb=tb: e.tensor_tensor(out=um[:, fc, tb * 128:(tb + 1) * 128], in0=mixed[:],
                                                                              in1=u[:, fc, tb * 128:(tb + 1) * 128], op=OP.mult),
                             reads=["gm_mixed", ("gm_u", fc)], writes=[("gm_um", fc)])
                tt_t = (t * TG) // TT
                col0 = (t * TG) % TT
                for dc in range(KC):
                    py = next_ps()
                    proj_fm("gm%dwo" % j, dc, lambda kk: (um[:, kk, :], ("gm_um", kk)), GC, TG, py)
                    P.op("act", lambda e, dc=dc, py=py, col0=col0: e.activation(out=ybuf[:, dc, col0:col0 + TG], in_=psum[py][:, :TG], func=AF.Copy),
                         reads=["ps%d" % py], writes=[("y", dc)])
                if col0 + TG == TT:
                    residual_ln(l, 0, tt_t, lambda kc: (ybuf[:, kc, :], ("y", kc)))

        def pool_layer(l, j):
            areset()
            wstr_alloc()
            zb = ar("pl_z%d" % j, [128, KC, 16 + L], F32)
            wk = ar("pl_wk%d" % j, [128, 16 + L], F32)
            wk2 = ar("pl_wk2%d" % j, [128, 16 + L], F32)
            pooled = ar("pl_pooled%d" % j, [128, KC, TT], BF16)
            y1 = ar("pl_y1%d" % j, [128, KC, TT], BF16)
            hh = ar("pl_hh%d" % j, [128, KC, 16], BF16)
            halo_all = ar("pl_ha%d" % j, [128, NCORES, KC * 2 * 16], F32)
            halo_sel = ar("pl_hs%d" % j, [128, KC, 2, 16], F32)
            halo_loc = ar("pl_hl%d" % j, [128, KC, 2, 16], F32)
            ptmp = ar("pl_tmp%d" % j, [128, 16], F32)
            nfo, _ = cidx["not_first"]
            hdl = nc.dram_tensor("pl_hdl%d" % j, [128, KC * 2 * 16], F32).ap()
            hda = nc.dram_tensor("pl_hda%d" % j, [NCORES * 128, KC * 2 * 16], F32).ap()
            pso, _ = cidx["pool_scale"]
            rco, _ = cidx["pool_rc"]
            pho, _ = cidx["prev_onehot"]
            gd_chunks = KC // cfg.PG
            for kc in range(KC):
                for seg in range(2):
                    t_last = (seg * L + L - 16) // TT
                    P.op("pool", lambda e, kc=kc, seg=seg: e.tensor_copy(out=halo_loc[:, kc, seg, :], in_=S[:, kc, seg * L + L - 16: seg * L + L]),
                         reads=[("S", kc, t_last)], writes=["pl_hl"])
            P.op("act", lambda e: e.dma_start(out=hdl, in_=halo_loc[:].rearrange("p a b c -> p (a b c)")), reads=["pl_hl"], writes=["pl_hdl"], kind="d")
            P.op("pool", lambda e: e.collective_compute("AllGather", OP.bypass, replica_groups=groups, ins=[hdl], outs=[hda]),
                 reads=["pl_hdl"], writes=["pl_hda"], kind="cc")
            P.op("sp", lambda e: e.dma_start(out=halo_all[:], in_=hda.rearrange("(r p) f -> p r f", p=128)), reads=["pl_hda"], writes=["pl_ha"], kind="d")
            hs_flat = halo_sel[:].rearrange("p a b c -> p (a b c)")
            P.op("dve", lambda e: e.tensor_scalar(out=hs_flat, in0=halo_all[:, 0, :], scalar1=cst[:, pho:pho + 1], scalar2=None, op0=OP.mult),
                 reads=["pl_ha", "cst"], writes=["pl_hs"])
            for r in range(1, NCORES):
                P.op("dve", lambda e, r=r: e.scalar_tensor_tensor(out=hs_flat, in0=halo_all[:, r, :], scalar=cst[:, pho + r:pho + r + 1], in1=hs_flat,
                                                                   op0=OP.mult, op1=OP.add), reads=["pl_ha", "cst", "pl_hs"], writes=["pl_hs"])
            for seg in range(2):
                b = seg
                for kc in range(KC):
                    shcol = modcol(l, 0, kc, b)
                    m1 = dsc[:, l, 0, 0, kc, b:b + 1]
                    P.op("dve", lambda e, kc=kc, seg=seg, m1=m1, shcol=shcol: e.tensor_scalar(
                        out=hh[:, kc, :], in0=halo_sel[:, kc, seg, :], scalar1=m1, scalar2=shcol, op0=OP.mult, op1=OP.add),
                        reads=["pl_hs", "dsc", "modsb"], writes=["pl_hh"])
                for mc in range(KC):
                    pz = next_ps()
                    proj_fm("pl%dwi" % j, mc, lambda kk: (hh[:, kk, :], "pl_hh"), KC, 16, pz)
                    P.op("act", lambda e, mc=mc, pz=pz: e.activation(out=zb[:, mc, 0:16], in_=psum[pz][:, :16], func=AF.Copy, scale=cst[:, nfo:nfo + 1]),
                         reads=["ps%d" % pz, "cst"], writes=[("pl_z", mc)])
                for tl in range(L // TT):
                    t = seg * (L // TT) + tl
                    hi = make_h(l, 0, t)
                    for mc in range(KC):
                        pz = next_ps()
                        proj_fm("pl%dwi" % j, mc, lambda kk: (hbuf[hi][:, kk, :], ("h", hi, kk)), KC, TT, pz)
                        P.op("act", lambda e, mc=mc, pz=pz, tl=tl: e.activation(out=zb[:, mc, 16 + tl * TT: 16 + (tl + 1) * TT], in_=psum[pz][:, :TT], func=AF.Copy),
                             reads=["ps%d" % pz], writes=[("pl_z", mc)])
                for tl in range(L // TT):
                    t = seg * (L // TT) + tl
                    for mc in range(KC):
                        g = mc // gd_chunks
                        nsteps = g + 1
                        win = 2 ** nsteps
                        cur_ap = zb[:, mc, :]
                        bufs = [wk, wk2]
                        lo = 16 + tl * TT - 16
                        hi_ = 16 + (tl + 1) * TT
                        for st in range(nsteps):
                            sh = 2 ** st
                            dst = bufs[st % 2]
                            a0 = lo + 2 * sh - 1
                            P.op("dve" if (mc % 2 == 0) else "pool", lambda e, cur_ap=cur_ap, dst=dst, a0=a0, hi_=hi_, sh=sh: e.tensor_tensor(
                                out=dst[:, a0:hi_], in0=cur_ap[:, a0:hi_], in1=cur_ap[:, a0 - sh:hi_ - sh], op=OP.add),
                                reads=[("pl_z", mc), "pl_wk", "pl_wk2"], writes=["pl_wk" if st % 2 == 0 else "pl_wk2"])
                            cur_ap = dst[:]
                        c0 = 16 + tl * TT
                        P.op("dve", lambda e, cur_ap=cur_ap, mc=mc, c0=c0, win=win: e.scalar_tensor_tensor(
                            out=pooled[:, mc, :], in0=cur_ap[:, c0:c0 + TT], scalar=1.0 / win, in1=zb[:, mc, c0:c0 + TT], op0=OP.mult, op1=OP.subtract),
                            reads=["pl_wk", "pl_wk2", ("pl_z", mc)], writes=[("pl_pooled", mc)])
                        if tl == 0:
                            P.op("dve", lambda e, cur_ap=cur_ap, mc=mc, g=g: e.tensor_tensor(
                                out=ptmp[:, 0:16], in0=cur_ap[:, 16:32], in1=cst[:, rco + g * 16: rco + (g + 1) * 16], op=OP.mult),
                                reads=["pl_wk", "pl_wk2", "cst"], writes=["sil0"])
                            P.op("dve", lambda e, mc=mc: e.tensor_tensor(out=pooled[:, mc, 0:16], in0=ptmp[:, 0:16], in1=zb[:, mc, 16:32], op=OP.subtract),
                                 reads=["sil0", ("pl_z", mc)], writes=[("pl_pooled", mc)])
                    for g in range(cfg.PG):
                        for dcg in range(gd_chunks):
                            mc = g * gd_chunks + dcg
                            pg_ = next_ps()
                            proj_fm("pl%dwg" % j, mc, lambda kk, g=g: (pooled[:, g * gd_chunks + kk, :], ("pl_pooled", g * gd_chunks + kk)), gd_chunks, TT, pg_)
                            P.op("act", lambda e, mc=mc, pg_=pg_: e.activation(out=y1[:, mc, :], in_=psum[pg_][:, :TT], func=AF.Copy,
                                                                               scale=cst[:, pso + j * KC + mc: pso + j * KC + mc + 1]),
                                 reads=["ps%d" % pg_, "cst"], writes=[("pl_y1", mc)])
                    for dc in range(KC):
                        py = next_ps()
                        proj_fm("pl%dwo" % j, dc, lambda kk: (y1[:, kk, :], ("pl_y1", kk)), KC, TT, py)
                        P.op("act", lambda e, dc=dc, py=py: e.activation(out=ybuf[:, dc, :], in_=psum[py][:, :TT], func=AF.Copy),
                             reads=["ps%d" % py], writes=[("y", dc)])
                    residual_ln(l, 0, t, lambda kc: (ybuf[:, kc, :], ("y", kc)))

        def fox_layer(l, j):
            H, HD = cfg.H, cfg.HD
            NB = L // 128
            NQ = L // TT
            NBT = T // 128
            KA = min(4, KC)
            n_ag = KC // KA
            qscale = HD ** -0.5
            qd = nc.dram_tensor("fx_q%d" % j, [D, T], BF16).ap()

            def agpair(name, rows, cols):
                nw = rows * cols
                cc = None
                for c_ in (2048, 1024, 512, 256, 128, 64, 32):
                    if nw % c_ == 0 and nw // c_ <= 256:
                        cc = c_
                        break
                assert cc is not None, (name, rows, cols)
                rr = nw // cc
                i_raw = nc.dram_tensor(name + "_l", [rr, cc], F32).ap()
                o_raw = nc.dram_tensor(name + "_a", [NCORES * rr, cc], F32).ap()
                i_v = i_raw.rearrange("a b -> (a b)").rearrange("(x y) -> x y", y=cols)
                o_v = o_raw.rearrange("a b -> (a b)").rearrange("(x y) -> x y", y=cols)
                return i_raw, o_raw, i_v, o_v
            kp = [agpair("fx_k%d_%d" % (j, a), KA * 128, T // 2) for a in range(n_ag)]
            vp = [agpair("fx_v%d_%d" % (j, s_), L, D // 2) for s_ in range(2)]
            cp_ = agpair("fx_c%d" % j, 128, T)
            kloc_raw, kall_raw = [p_[0] for p_ in kp], [p_[1] for p_ in kp]
            kloc, kall = [p_[2] for p_ in kp], [p_[3] for p_ in kp]
            vloc_raw, vall_raw = [p_[0] for p_ in vp], [p_[1] for p_ in vp]
            vloc, vall = [p_[2] for p_ in vp], [p_[3] for p_ in vp]
            cloc_raw, call_raw, cloc, call = cp_
            cqd = nc.dram_tensor("fx_cq%d" % j, [128, 3 * T], BF16).ap()
            nbd = nc.dram_tensor("fx_nb%d" % j, [128, NCORES * NBT * H], F32).ap()
            atd = nc.dram_tensor("fx_at%d" % j, [D, T], BF16).ap()
            kloc_bf = [k_.bitcast(BF16) for k_ in kloc]
            kall_bf = [k_.bitcast(BF16) for k_ in kall]
            vloc_bf = [v_.bitcast(BF16) for v_ in vloc]
            vall_bf = [v_.bitcast(BF16) for v_ in vall]
            bfo, _ = cidx["fox_bf"]
            oco, _ = cidx["one_col"]
            mlt, _ = cidx["m_lt"]
            mgt, _ = cidx["m_gtbig"]
            meq, _ = cidx["m_eq"]
            mne, _ = cidx["m_neq"]
            nbe, _ = cidx["negbig_eq"]
            areset()
            wstr_alloc()
            qst = ar("fx_qst", [128, KC, TT], BF16)
            kst = ar("fx_kst", [128, KC, TT], BF16)
            vst = ar("fx_vst", [128, TT // 128, D], BF16)
            wvf = ar("fx_wvf", [128, KC * 512], BF16)
            fa = ar("fx_fa", [128, TT], F32)
            fe = ar("fx_fe", [128, TT], F32)
            fl = ar("fx_fl", [128, TT], F32)
            ones_t = ar("fx_ones", [128, TT], F32)
            cumT = ar("fx_cumT", [128, T], F32)
            P.op("pool", lambda e: e.memset(ones_t[:], 1.0), writes=["fx_ones"])
            for t in range(NT):
                seg = (t * TT) // L
                tl = t - seg * NQ
                hi = make_h(l, 0, t)
                for mc in range(KC):
                    pq = next_ps()
                    proj_fm("fx%dwq" % j, mc, lambda kk: (hbuf[hi][:, kk, :], ("h", hi, kk)), KC, TT, pq)
                    P.op("act", lambda e, mc=mc, pq=pq: e.activation(out=qst[:, mc, :], in_=psum[pq][:, :TT], func=AF.Copy, scale=qscale),
                         reads=["ps%d" % pq], writes=["fx_qst"])
                    pk_ = next_ps()
                    proj_fm("fx%dwk" % j, mc, lambda kk: (hbuf[hi][:, kk, :], ("h", hi, kk)), KC, TT, pk_)
                    P.op("dve", lambda e, mc=mc, pk_=pk_: e.tensor_copy(out=kst[:, mc, :], in_=psum[pk_][:, :TT]),
                         reads=["ps%d" % pk_], writes=["fx_kst"])
                P.op("act", lambda e, t=t: e.dma_start(out=qd.rearrange("(kc p) t -> p kc t", p=128)[:, :, t * TT:(t + 1) * TT], in_=qst[:]),
                     reads=["fx_qst"], writes=["fx_q"], kind="d")
                for a in range(n_ag):
                    P.op("act", lambda e, t=t, a=a: e.dma_start(out=kloc_bf[a].rearrange("(kc p) t -> p kc t", p=128)[:, :, t * TT:(t + 1) * TT],
                                                                   in_=kst[:, a * KA:(a + 1) * KA, :]),
                         reads=["fx_kst"], writes=["fx_kl%d" % a], kind="d")
                for cg in range(D // 512):
                    src, res, free = wblock("fx%dwv" % j, cg)
                    P.op("sp", lambda e, src=src: e.dma_start(out=wvf[:], in_=src), reads=[res], writes=["fx_wvf"], kind="d")
                    for tb in range(TT // 128):
                        pv = next_ps()
                        for kc in range(KC):
                            P.op("pe", lambda e, kc=kc, tb=tb, pv=pv, hi=hi: e.matmul(psum[pv][:, :512], hbuf[hi][:, kc, tb * 128:(tb + 1) * 128],
                                                                                      wvf[:, kc * 512:(kc + 1) * 512], start=(kc == 0), stop=(kc == KC - 1)),
                                 reads=["fx_wvf", ("h", hi, kc)], writes=["ps%d" % pv])
                        P.op("act", lambda e, tb=tb, cg=cg, pv=pv: e.activation(out=vst[:, tb, cg * 512:(cg + 1) * 512], in_=psum[pv][:, :512], func=AF.Copy),
                             reads=["ps%d" % pv], writes=["fx_vst"])
                P.op("act", lambda e, seg=seg, tl=tl: e.dma_start(
                    out=vloc_bf[seg][tl * TT:(tl + 1) * TT, :].rearrange("(tb p) d -> p tb d", p=128), in_=vst[:]),
                    reads=["fx_vst"], writes=["fx_vl%d" % seg], kind="d")
                pf = next_ps()
                proj_fm("fx%dwf" % j, 0, lambda kk: (hbuf[hi][:, kk, :], ("h", hi, kk)), KC, TT, pf)
                P.op("act", lambda e, pf=pf: e.activation(out=fa[:], in_=psum[pf][:, :TT], func=AF.Identity, bias=cst[:, bfo + j:bfo + j + 1]),
                     reads=["ps%d" % pf, "cst"], writes=["fx_fa"])
                P.op("act", lambda e: e.activation(out=fe[:], in_=fa[:], func=AF.Abs), reads=["fx_fa"], writes=["fx_fe"])
                P.op("act", lambda e: e.activation(out=fe[:], in_=fe[:], func=AF.Exp, scale=-1.0), reads=["fx_fe"], writes=["fx_fe"])
                P.op("act", lambda e: e.activation(out=fl[:], in_=fe[:], func=AF.Ln, bias=cst[:, oco:oco + 1]), reads=["fx_fe", "cst"], writes=["fx_fl"])
                P.op("dve", lambda e: e.scalar_tensor_tensor(out=fa[:], in0=fa[:], scalar=0.0, in1=fl[:], op0=OP.min, op1=OP.subtract),
                     reads=["fx_fa", "fx_fl"], writes=["fx_fa"])
                init = 0.0 if tl == 0 else cumT[:, t * TT - 1:t * TT]
                P.op("dve", lambda e, t=t, init=init: e.tensor_tensor_scan(out=cumT[:, t * TT:(t + 1) * TT], data0=ones_t[:], data1=fa[:], initial=init,
                                                                             op0=OP.mult, op1=OP.add),
                     reads=["fx_fa", "fx_ones", "fx_cumT"], writes=["fx_cumT"])
            P.op("act", lambda e: e.dma_start(out=cloc, in_=cumT[:]), reads=["fx_cumT"], writes=["fx_cl"], kind="d")
            for a in range(n_ag):
                P.op("pool", lambda e, a=a: e.collective_compute("AllGather", OP.bypass, replica_groups=groups, ins=[kloc_raw[a]], outs=[kall_raw[a]]),
                     reads=["fx_kl%d" % a], writes=["fx_ka%d" % a], kind="cc")
            for s_ in range(2):
                P.op("pool", lambda e, s_=s_: e.collective_compute("AllGather", OP.bypass, replica_groups=groups, ins=[vloc_raw[s_]], outs=[vall_raw[s_]]),
                     reads=["fx_vl%d" % s_], writes=["fx_va%d" % s_], kind="cc")
            P.op("pool", lambda e: e.collective_compute("AllGather", OP.bypass, replica_groups=groups, ins=[cloc_raw], outs=[call_raw]),
                 reads=["fx_cl"], writes=["fx_ca"], kind="cc")
            areset()
            crt = [ar("fx_crt%d" % i, [128, T], F32) for i in range(2)]
            nbT = ar("fx_nbT", [128, T], F32)
            nb = ar("fx_nb", [128, NCORES, NBT, H], F32)
            gsm = ar("fx_gsm", [128, 8], F32)
            cqs = ar("fx_cqs", [128, 3, T], BF16)
            cq32 = ar("fx_cq32", [128, T], F32)
            rem = ar("fx_rem", [128, T], F32)
            own = ar("fx_own", [128, T], F32)
            P.op("pool", lambda e: e.memset(gsm[:], 0.0), writes=["fx_gsm"])
            P.op("sp", lambda e: e.dma_start(out=own[:], in_=cloc), reads=["fx_cl"], writes=["fx_own"], kind="d")
            for r in range(NCORES):
                ci = r % 2
                P.op("sp", lambda e, r=r, ci=ci: e.dma_start(out=crt[ci][:], in_=call[r * 128:(r + 1) * 128, :]), reads=["fx_ca"], writes=["fx_crt%d" % ci], kind="d")
                for b in range(2):
                    P.op("dve", lambda e, r=r, b=b: e.tensor_tensor(out=gsm[:, 4 + b:5 + b], in0=gsm[:, b:b + 1], in1=cst[:, mgt + r:mgt + r + 1], op=OP.add),
                         reads=["fx_gsm", "cst"], writes=["fx_gsm"])
                    P.op("dve", lambda e, b=b, ci=ci: e.tensor_scalar(out=nbT[:, b * L:(b + 1) * L], in0=crt[ci][:, b * L:(b + 1) * L], scalar1=gsm[:, 4 + b:5 + b],
                                                                       scalar2=-1.0, op0=OP.add, op1=OP.mult),
                         reads=["fx_gsm", "fx_crt%d" % ci], writes=["fx_nbT"])
                for blk in range(NBT):
                    pt_ = next_ps()
                    P.op("pe", lambda e, blk=blk, pt_=pt_: e.transpose(psum[pt_][:, :128], nbT[:, blk * 128:(blk + 1) * 128], C("ident")),
                         reads=["fx_nbT", "cst"], writes=["ps%d" % pt_])
                    P.op("act", lambda e, r=r, blk=blk, pt_=pt_: e.activation(out=nb[:, r, blk, :], in_=psum[pt_][:, :H], func=AF.Copy),
                         reads=["ps%d" % pt_], writes=["fx_nb"])
                for b in range(2):
                    col = b * L + L - 1
                    P.op("dve", lambda e, r=r, b=b, ci=ci, col=col: e.scalar_tensor_tensor(out=gsm[:, 2 + b:3 + b], in0=crt[ci][:, col:col + 1],
                                                                                          scalar=cst[:, mlt + r:mlt + r + 1], in1=gsm[:, 2 + b:3 + b], op0=OP.mult, op1=OP.add),
                         reads=["fx_gsm", "cst", "fx_crt%d" % ci], writes=["fx_gsm"])
                    P.op("dve", lambda e, b=b, ci=ci, col=col: e.tensor_tensor(out=gsm[:, b:b + 1], in0=gsm[:, b:b + 1], in1=crt[ci][:, col:col + 1], op=OP.add),
                         reads=["fx_gsm", "fx_crt%d" % ci], writes=["fx_gsm"])
            P.op("act", lambda e: e.dma_start(out=nbd, in_=nb[:].rearrange("p a b c -> p (a b c)")), reads=["fx_nb"], writes=["fx_nbd"], kind="d")
            for b in range(2):
                P.op("dve", lambda e, b=b: e.tensor_scalar(out=cq32[:, b * L:(b + 1) * L], in0=own[:, b * L:(b + 1) * L], scalar1=gsm[:, 2 + b:3 + b], scalar2=None, op0=OP.add),
                     reads=["fx_own", "fx_gsm"], writes=["fx_cq32"])
            P.op("dve", lambda e: e.tensor_copy(out=cqs[:, 0, :], in_=cq32[:]), reads=["fx_cq32"], writes=["fx_cqs0"])
            P.op("dve", lambda e: e.tensor_tensor(out=rem[:], in0=cq32[:], in1=cqs[:, 0, :], op=OP.subtract), reads=["fx_cq32", "fx_cqs0"], writes=["fx_rem"])
            P.op("dve", lambda e: e.tensor_copy(out=cqs[:, 1, :], in_=rem[:]), reads=["fx_rem"], writes=["fx_cqs1"])
            P.op("dve", lambda e: e.tensor_tensor(out=cq32[:], in0=rem[:], in1=cqs[:, 1, :], op=OP.subtract), reads=["fx_rem", "fx_cqs1"], writes=["fx_cq32"])
            P.op("dve", lambda e: e.tensor_copy(out=cqs[:, 2, :], in_=cq32[:]), reads=["fx_cq32"], writes=["fx_cqs2"])
            P.op("act", lambda e: e.dma_start(out=cqd, in_=cqs[:].rearrange("p a b -> p (a b)")), reads=["fx_cqs0", "fx_cqs1", "fx_cqs2"], writes=["fx_cqd"], kind="d")
            areset()
            Ka = ar("fx_Ka", [128, NCORES * L], BF16)
            Va = ar("fx_Va", [128, NCORES * NB, 128], BF16)
            Qa = ar("fx_Qa", [128, L], BF16)
            nb3 = ar("fx_nb3", [128, NCORES, NBT, H], F32)
            caus = ar("fx_caus", [128, TT // 128, TT], F32)
            Pt = [ar("fx_Pt%d" % i, [128, TT], BF16) for i in range(3)]
            tmpx = [ar("fx_tmpx%d" % i, [128, TT], F32) for i in range(2)]
            rsb = [ar("fx_rsb%d" % i, [128, TT], F32) for i in range(2)]
            nsb = ar("fx_nsb", [128, TT], F32)
            ao = ar("fx_ao", [128, TT], BF16)
            P.op("sp", lambda e: e.dma_start(out=nb3[:].rearrange("p a b c -> p (a b c)"), in_=nbd), reads=["fx_nbd"], writes=["fx_nb3"], kind="d")
            for oi in range(TT // 128):
                load_big("negc%d" % (oi * 128), caus[:, oi, :], ("fx_caus", oi))
            P.op("pool", lambda e: e.memset(rsb[0][:], 0.0), writes=["fx_rsb0"])
            P.op("pool", lambda e: e.memset(rsb[1][:], 0.0), writes=["fx_rsb1"])
            P.op("pool", lambda e: e.memset(Ka[64:67, :], 1.0), writes=["fx_Ka1"])
            pcount = 0
            for b in range(2):
                for h in range(H):
                    par = h % 2
                    kc_h = h // 2
                    a_, kcl = kc_h // KA, kc_h % KA
                    r0 = kcl * 128 + par * 64
                    P.op("sp", lambda e, a_=a_, r0=r0, b=b: e.dma_start(
                        out=Ka[0:64, :].rearrange("p (r t) -> p r t", r=NCORES),
                        in_=kall_bf[a_].rearrange("(r x) t -> x r t", r=NCORES)[r0:r0 + 64, :, b * L:(b + 1) * L]),
                        reads=["fx_ka%d" % a_], writes=["fx_Ka"], kind="d")
                    P.op("sp", lambda e, h=h, b=b: e.dma_start(out=Qa[0:64, :], in_=qd[h * 64:(h + 1) * 64, b * L:(b + 1) * L]),
                         reads=["fx_q"], writes=["fx_Qa"], kind="d", semkey="fx_Qa_a")
                    P.op("sp", lambda e, h=h, b=b: e.dma_start(out=Qa[64:67, :], in_=cqd[h:h + 1, :].rearrange("o (c t) -> (o c) t", c=3)[:, b * L:(b + 1) * L]),
                         reads=["fx_cqd"], writes=["fx_Qa2"], kind="d")
                    vo = 0 if par == 0 else 64
                    oo = 64 - vo
                    for rr_ in range(NCORES):
                        P.op("sp", lambda e, h=h, b=b, vo=vo, rr_=rr_: e.dma_start(
                            out=Va[:, rr_ * NB:(rr_ + 1) * NB, vo:vo + 64],
                            in_=vall_bf[b].rearrange("(blk p) d -> p blk d", p=128)[:, rr_ * NB:(rr_ + 1) * NB, h * 64:(h + 1) * 64]),
                            reads=["fx_va%d" % b], writes=[("fx_Va", rr_)], kind="d")
                    P.op("pool", lambda e, oo=oo: e.memset(Va[:, :, oo:oo + 64], 1.0), writes=["fx_Va1"])
                    dlo = 64 if par == 0 else 0
                    nlo = 0 if par == 0 else 64
                    for qi in range(NQ):
                        po = next_ps()
                        ps_reserved.add(po)
                        nblk = NCORES * NB
                        idx = 0
                        for r in range(NCORES):
                            for kb in range(NB):
                                kcol = r * L + kb * 128
                                ps_ = next_ps()
                                P.op("pe", lambda e, ps_=ps_, kcol=kcol, qi=qi: e.matmul(psum[ps_][:, :TT], Ka[0:67, kcol:kcol + 128], Qa[0:67, qi * TT:(qi + 1) * TT],
                                                                                         start=True, stop=True),
                                     reads=["fx_Ka", "fx_Ka1", "fx_Qa", "fx_Qa2"], writes=["ps%d" % ps_])
                                pi_ = pcount % 3
                                pcount += 1
                                src_ap, src_res = psum[ps_][:, :TT], "ps%d" % ps_
                                if kb * 128 >= (qi + 1) * TT:
                                    tx = pcount % 2
                                    P.op("dve", lambda e, ps_=ps_, tx=tx, r=r: e.tensor_scalar(out=tmpx[tx][:], in0=psum[ps_][:, :TT], scalar1=cst[:, nbe + r:nbe + r + 1],
                                                                                           scalar2=None, op0=OP.add),
                                         reads=["ps%d" % ps_, "cst"], writes=[("fx_tmpx", tx)])
                                    src_ap, src_res = tmpx[tx][:], ("fx_tmpx", tx)
                                elif kb * 128 + 127 > qi * TT:
                                    oi = (kb * 128 - qi * TT) // 128
                                    tx = pcount % 2
                                    P.op("dve", lambda e, ps_=ps_, tx=tx, r=r, oi=oi: e.scalar_tensor_tensor(
                                        out=tmpx[tx][:], in0=caus[:, oi, :], scalar=cst[:, meq + r:meq + r + 1], in1=psum[ps_][:, :TT], op0=OP.mult, op1=OP.add),
                                        reads=["ps%d" % ps_, "cst", ("fx_caus", oi)], writes=[("fx_tmpx", tx)])
                                    src_ap, src_res = tmpx[tx][:], ("fx_tmpx", tx)
                                P.op("act", lambda e, src_ap=src_ap, pi_=pi_, r=r, kb=kb, b=b, h=h: e.activation(
                                    out=Pt[pi_][:], in_=src_ap, func=AF.Exp, bias=nb3[:, r, b * NB + kb, h:h + 1]),
                                    reads=[src_res, "fx_nb3"], writes=[("fx_Pt", pi_)])
                                P.op("pe", lambda e, po=po, r=r, kb=kb, pi_=pi_, idx=idx, nblk=nblk: e.matmul(
                                    psum[po][:, :TT], Va[:, r * NB + kb, :], Pt[pi_][:], start=(idx == 0), stop=(idx == nblk - 1)),
                                    reads=[("fx_Va", r), "fx_Va1", ("fx_Pt", pi_)], writes=["ps%d" % po])
                                idx += 1
                        P.op("dve", lambda e, po=po, par=par, dlo=dlo: e.reciprocal(out=rsb[par][dlo:dlo + 64, :], in_=psum[po][dlo:dlo + 64, :TT]),
                             reads=["ps%d" % po], writes=["fx_rsb%d" % par])
                        P.op("act", lambda e, po=po, nlo=nlo: e.activation(out=nsb[nlo:nlo + 64, :], in_=psum[po][nlo:nlo + 64, :TT], func=AF.Copy),
                             reads=["ps%d" % po], writes=["fx_nsb"])
                        ps_reserved.discard(po)
                        pw = next_ps()
                        P.op("pe", lambda e, pw=pw, par=par: e.matmul(psum[pw][:, :TT], C("swap"), rsb[par][:], start=True, stop=True),
                             reads=["fx_rsb%d" % par, "cst"], writes=["ps%d" % pw])
                        P.op("dve", lambda e, pw=pw, nlo=nlo: e.tensor_tensor(out=ao[nlo:nlo + 64, :], in0=nsb[nlo:nlo + 64, :], in1=psum[pw][nlo:nlo + 64, :TT], op=OP.mult),
                             reads=["fx_nsb", "ps%d" % pw], writes=["fx_ao"])
                        c0 = b * L + qi * TT
                        P.op("act", lambda e, h=h, nlo=nlo, c0=c0: e.dma_start(out=atd[h * 64:(h + 1) * 64, c0:c0 + TT], in_=ao[nlo:nlo + 64, :]),
                             reads=["fx_ao"], writes=["fx_at"], kind="d")
            areset()
            wstr_alloc()
            att = ar("fx_att", [128, KC, TT], BF16)
            for t in range(NT):
                P.op("sp", lambda e, t=t: e.dma_start(out=att[:], in_=atd.rearrange("(kc p) t -> p kc t", p=128)[:, :, t * TT:(t + 1) * TT]),
                     reads=["fx_at"], writes=["fx_att"], kind="d")
                for dc in range(KC):
                    py = next_ps()
                    proj_fm("fx%dwo" % j, dc, lambda kk: (att[:, kk, :], "fx_att"), KC, TT, py)
                    P.op("act", lambda e, dc=dc, py=py: e.activation(out=ybuf[:, dc, :], in_=psum[py][:, :TT], func=AF.Copy),
                         reads=["ps%d" % py], writes=[("y", dc)])
                residual_ln(l, 0, t, lambda kc: (ybuf[:, kc, :], ("y", kc)))

        nlayers = cfg.DEPTH if cfg.stop is None else cfg.stop
        for l in range(nlayers):
            kind, j = l % 3, l // 3
            if kind == 0:
                gmlp_layer(l, j)
            elif kind == 1:
                if not getattr(cfg, 'nofox', False):
                    fox_layer(l, j)
                else:
                    for t in range(NT):
                        P.op('pool', lambda e: e.memset(ybuf[:], 0.0), writes=[('y', kc) for kc in range(KC)])
                        residual_ln(l, 0, t, lambda kc: (ybuf[:, kc, :], ('y', kc)))
            else:
                pool_layer(l, j)
            if l % 2 == 0:
                dense_ffn_layer(l, l // 2)
            else:
                moe_layer(l, l // 2)

        last = None
        for kc in range(KC):
            for t in range(NT):
                P.op("pool", lambda e, kc=kc, t=t: e.tensor_scalar(out=S[:, kc, t * TT:(t + 1) * TT], in0=S[:, kc, t * TT:(t + 1) * TT],
                                                                    scalar1=1.0 / cfg.alpha, scalar2=None, op0=OP.mult),
                     reads=[("S", kc, t)], writes=[("S", kc, t)])
            last = P.op("act", lambda e, kc=kc: e.dma_start(out=outT[kc * 128:(kc + 1) * 128, :], in_=S[:, kc, :]),
                        reads=[("S", kc, t) for t in range(NT)], writes=["outT"], kind="d", semkey="outT")
        P.final_wait("act", last)
        P.emit(stack)
    return nc


def prepare(cfg, inp):
    flat, nch, widx = pack_weights(cfg, inp)
    in_maps = []
    ncol = 6 * cfg.D // NCORES
    nq = ncol // 128
    fl = flat.reshape(nch, NCORES, PIECE)
    cidx = None
    for c in range(NCORES):
        x0 = inp["x"][0, c * cfg.L:(c + 1) * cfg.L]
        x1 = inp["x"][1, c * cfg.L:(c + 1) * cfg.L]
        xT = np.ascontiguousarray(np.concatenate([x0, x1], 0).T)
        wsh = np.ascontiguousarray(fl[:, c, :]).reshape(nch * 2 * CH_ROWS, CHW)
        mw = np.stack([stat_blocks(inp["mod_w"][l][:, c * ncol:(c + 1) * ncol], cfg.KC) for l in range(cfg.DEPTH)], 0)
        mw = np.ascontiguousarray(mw).reshape(cfg.DEPTH * nq * 128, cfg.KC * 128)
        cst, cidx = pack_consts(cfg, inp, c)
        in_maps.append({"xT": xT, "wsh": wsh, "modw": mw, "cst": np.ascontiguousarray(cst)})
    return in_maps, nch, widx, cidx, in_maps[0]["cst"].shape[1]


def assemble(cfg, results):
    out = np.zeros((2, cfg.SEQ, cfg.D), np.float32)
    for c in range(NCORES):
        o = np.asarray(results[c]["outT"]).T
        out[0, c * cfg.L:(c + 1) * cfg.L] = o[:cfg.L]
        out[1, c * cfg.L:(c + 1) * cfg.L] = o[cfg.L:]
    return out


def kernel(**inputs):
    cfg = Cfg()
    inp = {k: np.asarray(v) for k, v in inputs.items()}
    in_maps, nch, widx, cidx, ncst = prepare(cfg, inp)
    nc = build_program(cfg, nch, widx, cidx, ncst)
    res = run_bass_kernel_spmd(nc, in_maps, core_ids=list(range(NCORES)))
    return assemble(cfg, res.results)
```

```python
import numpy as np
import concourse.bass as bass
import concourse.mybir as mybir
from concourse.bass_utils import run_bass_kernel_spmd

F32 = mybir.dt.float32
BF16 = mybir.dt.bfloat16
AF = mybir.ActivationFunctionType
OP = mybir.AluOpType
NCORES = 8
CHW = 2048
CH_ROWS = 256
CH_ELEMS = NCORES * CH_ROWS * CHW * 2
PIECE = CH_ROWS * CHW * 2
BIG = 30000.0


class Cfg:
    def __init__(self, D=1024, SEQ=8192, F=3584, TT=512, TTG=256, E=8, HD=64, GMG=8, PG=4, DEPTH=4, stop=None):
        self.D, self.SEQ, self.F, self.TT, self.TTG, self.E, self.HD = D, SEQ, F, TT, TTG, E, HD
        self.GMG, self.PG, self.DEPTH = GMG, PG, DEPTH
        self.B = 2
        self.L = SEQ // NCORES
        self.T = 2 * self.L
        self.KC = D // 128
        self.FC = F // 128
        self.H = D // HD
        self.GMW = 2 * D
        self.GC = self.GMW // 128
        self.alpha = (2 * DEPTH) ** 0.25
        self.stop = stop
        self.n_gm = (DEPTH + 2) // 3
        self.n_fox = (DEPTH + 1) // 3
        self.n_pool = DEPTH // 3
        self.n_dense = (DEPTH + 1) // 2
        self.n_moe = DEPTH // 2


class Packer:
    def __init__(self):
        self.parts = []
        self.pos = 0
        self.index = {}

    def add(self, name, blocks):
        nblk, p, free = blocks.shape
        assert p == 128
        bsz = 128 * free
        offs = []
        for k in range(nblk):
            if (self.pos % CH_ELEMS) + bsz > CH_ELEMS:
                pad = CH_ELEMS - (self.pos % CH_ELEMS)
                self.parts.append(np.zeros(pad, np.float32))
                self.pos += pad
            offs.append(self.pos)
            self.parts.append(np.ascontiguousarray(blocks[k]).reshape(-1))
            self.pos += bsz
        self.index[name] = (offs, free)

    def finish(self):
        pad = (-self.pos) % CH_ELEMS
        if pad:
            self.parts.append(np.zeros(pad, np.float32))
            self.pos += pad
        flat = np.concatenate(self.parts)
        nch = self.pos // CH_ELEMS
        return flat, nch


def stat_blocks(w, KCn):
    K, M = w.shape
    a = w.reshape(KCn, 128, M // 128, 128)
    return np.ascontiguousarray(a.transpose(2, 1, 0, 3)).reshape(M // 128, 128, KCn * 128)


def mov_blocks(w, KCn, ncol):
    K, M = w.shape
    a = w.reshape(KCn, 128, M // ncol, ncol)
    return np.ascontiguousarray(a.transpose(2, 1, 0, 3)).reshape(M // ncol, 128, KCn * ncol)


def pack_weights(cfg, inp):
    pk = Packer()
    D, F, KC, FC = cfg.D, cfg.F, cfg.KC, cfg.FC

    def add_ffn(tag, w13, w2):
        a = stat_blocks(w13[:, :F], KC).reshape(FC, 128, 1, KC * 128)
        b = stat_blocks(w13[:, F:], KC).reshape(FC, 128, 1, KC * 128)
        pk.add(tag + "w13", np.concatenate([a, b], axis=2).reshape(FC, 128, 2 * KC * 128))
        pk.add(tag + "w2", stat_blocks(w2, FC))

    def add_gm(j):
        w_in = inp["gm_w_in"][j]
        pk.add(f"gm{j}wu", stat_blocks(w_in[:, :cfg.GMW], KC))
        pk.add(f"gm{j}wv", mov_blocks(w_in[:, cfg.GMW:], KC, 512))
        ws = inp["gm_w_s"][j]
        wst = np.ascontiguousarray(ws.transpose(2, 0, 1)).reshape(1, 128, cfg.GMG * 128)
        pk.add(f"gm{j}ws", wst)
        pk.add(f"gm{j}wo", stat_blocks(inp["gm_w_out"][j], cfg.GC))

    def add_fox(j):
        w = inp["fox_w_in"][j]
        pk.add(f"fx{j}wq", stat_blocks(w[:, :D], KC))
        pk.add(f"fx{j}wk", stat_blocks(w[:, D:2 * D], KC))
        pk.add(f"fx{j}wv", mov_blocks(w[:, 2 * D:3 * D], KC, 512))
        wf = np.zeros((D, 128), np.float32)
        wf[:, :cfg.H] = w[:, 3 * D:]
        pk.add(f"fx{j}wf", stat_blocks(wf, KC))
        pk.add(f"fx{j}wo", stat_blocks(inp["fox_w_out"][j], KC))

    def add_pool(j):
        pk.add(f"pl{j}wi", stat_blocks(inp["pool_w_in"][j], KC))
        wg = inp["pool_w_grp"][j]
        gd = wg.shape[1]
        blks = [stat_blocks(wg[g], gd // 128) for g in range(cfg.PG)]
        pk.add(f"pl{j}wg", np.concatenate(blks, axis=0))
        pk.add(f"pl{j}wo", stat_blocks(inp["pool_w_out"][j], KC))

    for i in range(cfg.DEPTH):
        kind, j = i % 3, i // 3
        if kind == 0:
            add_gm(j)
        elif kind == 1:
            add_fox(j)
        else:
            add_pool(j)
        if i % 2 == 0:
            add_ffn(f"ff{i // 2}", inp["ffn_w13"][i // 2], inp["ffn_w2"][i // 2])
        else:
            m = i // 2
            wr = np.zeros((D, 128), np.float32)
            wr[:, :cfg.E] = inp["moe_w_router"][m]
            pk.add(f"mo{m}wr", mov_blocks(wr, KC, 128))
            for e in range(cfg.E):
                add_ffn(f"mo{m}e{e}", inp["moe_w13"][m, e], inp["moe_w2"][m, e])
    flat, nch = pk.finish()
    return flat, nch, pk.index


class ConstPack:
    BIGNAMES = ("gm_bs", "sel", "negc")

    def __init__(self):
        self.items = []

    def add(self, name, arr):
        arr = np.asarray(arr, np.float32).reshape(128, -1)
        self.items.append((name, arr))

    def finish(self):
        small = [(n, a) for n, a in self.items if not n.startswith(self.BIGNAMES)]
        big = [(n, a) for n, a in self.items if n.startswith(self.BIGNAMES)]
        index, cols, pos = {}, [], 0
        for n, a in small + big:
            index[n] = (pos, a.shape[1])
            cols.append(a)
            pos += a.shape[1]
        index["__nsmall__"] = (sum(a.shape[1] for _, a in small), 0)
        return np.concatenate(cols, axis=1), index


def fm(v, n):
    return np.ascontiguousarray(np.asarray(v, np.float32).reshape(n, 128).T)


def pack_consts(cfg, inp, core):
    cp = ConstPack()
    KC = cfg.KC
    cp.add("ln_g", np.stack([fm(inp["ln_g"][l, s], KC) for l in range(cfg.DEPTH) for s in range(2)], 1))
    cp.add("ln_b", np.stack([fm(inp["ln_b"][l, s], KC) for l in range(cfg.DEPTH) for s in range(2)], 1))
    ncol = 6 * cfg.D // NCORES
    nq = ncol // 128
    mb = np.stack([fm(inp["mod_b"][l, core * ncol:(core + 1) * ncol], nq) for l in range(cfg.DEPTH)], 1)
    cp.add("mod_b", mb)
    cp.add("cT", np.stack([fm(inp["c"][b], KC) for b in range(2)], 2))
    cp.add("gm_ln_g", np.stack([fm(inp["gm_ln_g"][j], cfg.GC) for j in range(cfg.n_gm)], 1))
    cp.add("gm_ln_b", np.stack([fm(inp["gm_ln_b"][j], cfg.GC) for j in range(cfg.n_gm)], 1))
    cp.add("gm_bs", np.broadcast_to(inp["gm_b_s"].reshape(1, -1), (128, inp["gm_b_s"].size)))
    tri = (np.arange(128)[:, None] <= np.arange(128)[None, :]).astype(np.float32)
    cp.add("tri", tri)
    cp.add("ident", np.eye(128, dtype=np.float32))
    cp.add("one_col", np.ones((128, 1), np.float32))
    cp.add("eps_col", np.full((128, 1), 1e-5, np.float32))
    sw = np.zeros((128, 128), np.float32)
    for kk in range(128):
        sw[kk, (kk + 64) % 128] = 1.0
    cp.add("swap", sw)
    if cfg.n_pool:
        cp.add("pool_scale", np.stack([fm(inp["pool_scale"][j], KC) for j in range(cfg.n_pool)], 1))
        rc = np.zeros((cfg.PG, 16), np.float32)
        for g in range(cfg.PG):
            win = 2 ** (g + 1)
            pos = np.arange(16) + core * cfg.L
            rc[g] = 1.0 / np.minimum(pos + 1, win)
        cp.add("pool_rc", np.broadcast_to(rc.reshape(1, -1), (128, rc.size)))
        prev = np.zeros(NCORES, np.float32)
        if core > 0:
            prev[core - 1] = 1.0
        cp.add("prev_onehot", np.broadcast_to(prev[None], (128, NCORES)))
        cp.add("not_first", np.full((128, 1), float(core > 0), np.float32))
    if cfg.n_moe:
        cp.add("moe_br", np.broadcast_to(inp["moe_b_router"].reshape(1, -1), (128, inp["moe_b_router"].size)))
        sel = np.zeros((128, cfg.E, 128), np.float32)
        for e in range(cfg.E):
            sel[e, e, :] = 1.0
        cp.add("sel", sel)
    if cfg.n_fox:
        bf = np.zeros((128, cfg.n_fox), np.float32)
        bf[:cfg.H, :] = inp["fox_b_f"].T
        cp.add("fox_bf", bf)
        r = np.arange(NCORES)
        cp.add("m_lt", np.broadcast_to((r < core).astype(np.float32)[None], (128, NCORES)))
        cp.add("m_gtbig", np.broadcast_to(((r > core) * BIG).astype(np.float32)[None], (128, NCORES)))
        cp.add("m_eq", np.broadcast_to((r == core).astype(np.float32)[None], (128, NCORES)))
        cp.add("negbig_eq", np.broadcast_to((-BIG * (r == core)).astype(np.float32)[None], (128, NCORES)))
        cp.add("m_neq", np.broadcast_to((r != core).astype(np.float32)[None], (128, NCORES)))
        sh = np.zeros((128, 64), np.float32)
        for m in range(64):
            sh[64 + m, m] = 1.0
        cp.add("shift", sh)
        for o in range(0, cfg.TT, 128):
            pat = ((o + np.arange(128))[:, None] <= np.arange(cfg.TT)[None, :]).astype(np.float32)
            cp.add(f"negc{o}", -BIG * (1.0 - pat))
    return cp.finish()


class Prog:
    ENG = ["pe", "act", "dve", "pool", "sp"]

    def __init__(self, nc):
        self.nc = nc
        self.streams = {e: [] for e in self.ENG}
        self.res_w = {}
        self.res_r = {}
        self.sem_count = {}
        self.waited = {e: {} for e in self.ENG}
        self.ps_i = 0

    def _deps(self, eng, reads, writes, is_pe_mm):
        deps = {}

        def need(sv, src_eng):
            if sv is None:
                return
            sem, val = sv
            if is_pe_mm and src_eng == "pe" and sem[0] == "eng" and sem[1] == "pe":
                return
            if val > deps.get(sem, 0):
                deps[sem] = val
        for r in reads:
            w = self.res_w.get(r)
            if w:
                need(w[0], w[1])
        for w_ in writes:
            w = self.res_w.get(w_)
            if w:
                need(w[0], w[1])
            for rr in self.res_r.get(w_, []):
                need(rr[0], rr[1])
        out = []
        for sem, val in deps.items():
            if self.waited[eng].get(sem, 0) < val:
                self.waited[eng][sem] = val
                out.append((sem, val))
        return out

    def op(self, eng, fn, reads=(), writes=(), kind="c", semkey=None):
        is_pe = (eng == "pe" and kind == "c")
        waits = self._deps(eng, reads, writes, is_pe)
        if kind == "c":
            self.eng_ops = getattr(self, "eng_ops", {})
            n_ = self.eng_ops.get(eng, 0)
            self.eng_ops[eng] = n_ + 1
            sem = ("eng", eng, n_ // 20000)
            inc = 1
        elif kind == "d":
            sem = ("dma", semkey if semkey is not None else writes[0])
            inc = 16
        else:
            sem = ("cc", 0)
            inc = 1
        val = self.sem_count.get(sem, 0) + inc
        self.sem_count[sem] = val
        self.streams[eng].append((waits, fn, sem, inc))
        for r in reads:
            self.res_r.setdefault(r, []).append(((sem, val), eng))
        for w_ in writes:
            self.res_w[w_] = ((sem, val), eng)
            self.res_r[w_] = []
        return (sem, val)

    def barrier(self):
        for eng in self.ENG:
            waits = []
            for sem, cnt in self.sem_count.items():
                if sem[0] == "cc":
                    continue
                if self.waited[eng].get(sem, 0) < cnt:
                    self.waited[eng][sem] = cnt
                    waits.append((sem, cnt))
            if waits:
                self.streams[eng].append((waits, None, None, 0))
        self.res_w = {k: v for k, v in self.res_w.items() if v[0][0][0] == "cc"}
        self.res_r = {}

    def final_wait(self, eng, sv):
        self.streams[eng].append(([sv], None, None, 0))

    def emit(self, stack):
        nc = self.nc
        sems = {}
        for key in self.sem_count:
            sems[key] = stack.enter_context(nc.semaphore("s%d" % len(sems)))
        block = stack.enter_context(nc.Block())
        engs = {"pe": block.tensor, "act": block.scalar, "dve": block.vector, "pool": block.gpsimd, "sp": block.sync}

        def mk(ename):
            stream = self.streams[ename]

            def body(eng):
                for waits, fn, sem, inc in stream:
                    for (s, v) in waits:
                        eng.wait_ge(sems[s], v)
                    if fn is not None:
                        ins = fn(eng)
                        ins.then_inc(sems[sem], inc)
            return body
        for ename in self.ENG:
            engs[ename](mk(ename))


def build_program(cfg, nch, widx, cidx, ncst):
    nc = bass.Bass("TRN2", target_bir_lowering=False)
    D, KC, FC, TT, T, L, E = cfg.D, cfg.KC, cfg.FC, cfg.TT, cfg.T, cfg.L, cfg.E
    NT = T // TT
    ncol = 6 * D // NCORES
    nq = ncol // 128
    xT = nc.dram_tensor("xT", [D, T], F32, kind="ExternalInput").ap()
    wsh = nc.dram_tensor("wsh", [nch * 2 * CH_ROWS, CHW], F32, kind="ExternalInput").ap()
    modw = nc.dram_tensor("modw", [cfg.DEPTH * nq * 128, KC * 128], F32, kind="ExternalInput").ap()
    cst_d = nc.dram_tensor("cst", [128, ncst], F32, kind="ExternalInput").ap()
    outT = nc.dram_tensor("outT", [D, T], F32, kind="ExternalOutput").ap()
    wloc = [nc.dram_tensor("wloc%d" % i, [CH_ROWS, CHW], F32).ap() for i in range(2)]
    walls = [nc.dram_tensor("wall%d" % i, [NCORES * CH_ROWS, CHW], F32).ap() for i in range(nch)]
    walls_bf = [w_.bitcast(BF16).rearrange("a b -> (a b)") for w_ in walls]
    modloc = nc.dram_tensor("modloc", [128, cfg.DEPTH * nq * 2], F32).ap()
    modall = nc.dram_tensor("modall", [NCORES * 128, cfg.DEPTH * nq * 2], F32).ap()
    groups = [list(range(NCORES))]

    import contextlib
    with contextlib.ExitStack() as stack:
        P = Prog(nc)

        def sb(name, shape, dt):
            return stack.enter_context(nc.sbuf_tensor(name, shape, dt))

        def C(name):
            o, n = cidx[name]
            assert o + n <= cidx["__nsmall__"][0], name
            return cst[:, o:o + n]

        def load_big(name, dst, key):
            o, n = cidx[name]
            P.op("sp", lambda e: e.dma_start(out=dst, in_=cst_d[:, o:o + n]), writes=[key], kind="d")

        ARENA = 78 * 1024
        arena = sb("arena", [128, ARENA // 2], BF16)
        apos = [0]

        def areset():
            P.barrier()
            apos[0] = 0

        def ar(name, shape, dt):
            n = 1
            for d in shape[1:]:
                n *= d
            nb = n * (4 if dt == F32 else 2)
            nb = (nb + 31) // 32 * 32
            off = apos[0]
            assert off + nb <= ARENA, ("arena overflow", name, off, nb)
            apos[0] = off + nb
            v = arena[:, off // 2:(off + nb) // 2]
            if dt == F32:
                v = v.bitcast(F32)
            v = v[:, :n]
            if len(shape) == 3:
                v = v.rearrange("p (a b) -> p a b", a=shape[1])
            elif len(shape) == 4:
                v = v.rearrange("p (a b c) -> p a b c", a=shape[1], b=shape[2])
            return v

        nsmall = cidx["__nsmall__"][0]
        cst = sb("cst_sb", [128, nsmall], F32)
        S = sb("S", [128, KC, T], F32)
        modsb = sb("modsb", [128, NCORES, cfg.DEPTH, nq, 2], F32)
        ones_f = sb("ones_f", [128, 128], F32)
        ones_b = sb("ones_b", [128, 128], BF16)
        ident_b = sb("ident_b", [128, 128], BF16)
        psum = [stack.enter_context(nc.psum_tensor("ps%d" % i, [128, 512], F32)) for i in range(8)]

        ps_reserved = set()

        def next_ps():
            while True:
                i = P.ps_i
                P.ps_i = (i + 1) % 8
                if i not in ps_reserved:
                    return i

        P.op("sp", lambda e: e.dma_start(out=cst[:], in_=cst_d[:, 0:nsmall]), writes=["cst"], kind="d")
        P.op("pool", lambda e: e.memset(ones_f[:], 1.0 / D), writes=["ones_f"])
        P.op("pool", lambda e: e.memset(ones_b[:], 1.0), writes=["ones_b"])
        P.op("dve", lambda e: e.tensor_copy(out=ident_b[:], in_=C("ident")), reads=["cst"], writes=["ident_b"])

        apos[0] = 0
        silc = sb("silc", [128, KC, 2], F32)
        P.op("act", lambda e: e.activation(out=silc[:].rearrange("p a b -> p (a b)"), in_=C("cT"), func=AF.Silu),
             reads=["cst"], writes=["silc"])
        mw = [ar("mw%d" % i, [128, KC * 128], F32) for i in range(2)]
        mloc = sb("mloc", [128, cfg.DEPTH, nq, 2], F32)
        mbo, _ = cidx["mod_b"]
        for l in range(cfg.DEPTH):
            for qi in range(nq):
                idx = l * nq + qi
                s = idx % 2
                P.op("sp", lambda e, s=s, idx=idx: e.dma_start(out=mw[s][:], in_=modw[idx * 128:(idx + 1) * 128, :]),
                     writes=["mw%d" % s], kind="d")
                pi = next_ps()
                for kc in range(KC):
                    P.op("pe", lambda e, pi=pi, s=s, kc=kc: e.matmul(psum[pi][:, 0:2], mw[s][:, kc * 128:(kc + 1) * 128],
                                                                      silc[:, kc, :], start=(kc == 0), stop=(kc == KC - 1)),
                         reads=["mw%d" % s, "silc"], writes=["ps%d" % pi])
                bcol = cst[:, mbo + l * nq + qi: mbo + l * nq + qi + 1]
                P.op("dve", lambda e, pi=pi, l=l, qi=qi, bcol=bcol: e.tensor_scalar(
                    out=mloc[:, l, qi, :], in0=psum[pi][:, 0:2], scalar1=bcol, scalar2=None, op0=OP.add),
                    reads=["ps%d" % pi, "cst"], writes=["mloc"])
        P.op("act", lambda e: e.dma_start(out=modloc, in_=mloc[:].rearrange("p a b c -> p (a b c)")),
             reads=["mloc"], writes=["modloc"], kind="d")
        P.op("pool", lambda e: e.collective_compute("AllGather", OP.bypass, replica_groups=groups, ins=[modloc], outs=[modall]),
             reads=["modloc"], writes=["modall"], kind="cc")
        P.op("sp", lambda e: e.dma_start(out=modsb[:].rearrange("p r a b c -> p r (a b c)"),
                                         in_=modall.rearrange("(r p) f -> p r f", p=128)),
             reads=["modall"], writes=["modsb"], kind="d")

        stg = [ar("stg%d" % i, [128, CHW], F32) for i in range(3)]
        stb = [ar("stb%d" % i, [128, CHW], BF16) for i in range(3)]
        cast_engs = ["dve", "act", "pool"]
        k = 0
        for j in range(nch):
            wl = wloc[j % 2]
            wl_bf = wl.bitcast(BF16).rearrange("a (t b) -> (a t) b", t=2)
            for q in range(4):
                s = k % 3
                row0 = j * 2 * CH_ROWS + q * 128
                P.op("sp", lambda e, s=s, row0=row0: e.dma_start(out=stg[s][:], in_=wsh[row0:row0 + 128, :]),
                     writes=["stg%d" % s], kind="d")
                ce = cast_engs[k % 3]
                if ce == "act":
                    P.op("act", lambda e, s=s: e.activation(out=stb[s][:], in_=stg[s][:], func=AF.Copy),
                         reads=["stg%d" % s], writes=["stb%d" % s])
                else:
                    P.op(ce, lambda e, s=s: e.tensor_copy(out=stb[s][:], in_=stg[s][:]),
                         reads=["stg%d" % s], writes=["stb%d" % s])
                P.op("act", lambda e, s=s, q=q, wl_bf=wl_bf: e.dma_start(out=wl_bf[q * 128:(q + 1) * 128, :], in_=stb[s][:]),
                     reads=["stb%d" % s], writes=["wloc%d" % (j % 2)], kind="d", semkey="wloc%d_%d" % (j % 2, q))
                k += 1
            P.op("pool", lambda e, wl=wl, j=j: e.collective_compute(
                "AllGather", OP.bypass, replica_groups=groups, ins=[wl], outs=[walls[j]]),
                reads=["wloc%d" % (j % 2)], writes=["wall%d" % j], kind="cc")

        def wres(off):
            return "wall%d" % (off // CH_ELEMS)

        def wblock(name, kblk):
            offs, free = widx[name]
            off = offs[kblk]
            cj, o2 = off // CH_ELEMS, off % CH_ELEMS
            return walls_bf[cj][o2:o2 + 128 * free].rearrange("(p f) -> p f", p=128), wres(off), free

        def modcol(l, jcomp, kc, b):
            qg = jcomp * KC + kc
            return modsb[:, qg // nq, l, qg % nq, b:b + 1]

        dsc = sb("dsc", [128, cfg.DEPTH, 2, 2, KC, 2], F32)
        for l in range(cfg.DEPTH):
            for sub in range(2):
                for kc in range(KC):
                    P.op("pool", lambda e, l=l, sub=sub, kc=kc: e.tensor_scalar(
                        out=dsc[:, l, sub, 0, kc, :], in0=modsb[:, (((3 * sub + 1) * KC + kc) // nq), l, ((3 * sub + 1) * KC + kc) % nq, :],
                        scalar1=1.0, scalar2=1.0 / cfg.alpha, op0=OP.add, op1=OP.mult), reads=["modsb"], writes=["dsc"])
                    P.op("pool", lambda e, l=l, sub=sub, kc=kc: e.tensor_scalar(
                        out=dsc[:, l, sub, 1, kc, :], in0=modsb[:, (((3 * sub + 2) * KC + kc) // nq), l, ((3 * sub + 2) * KC + kc) % nq, :],
                        scalar1=1.0, scalar2=None, op0=OP.add), reads=["modsb"], writes=["dsc"])
        lng = sb("lng", [128, cfg.DEPTH * 2 * KC], F32)
        lnb = sb("lnb", [128, cfg.DEPTH * 2 * KC], F32)
        P.op("pool", lambda e: e.tensor_scalar(out=lng[:], in0=C("ln_g"), scalar1=cfg.alpha, scalar2=None, op0=OP.mult),
             reads=["cst"], writes=["lng"])
        P.op("pool", lambda e: e.tensor_scalar(out=lnb[:], in0=C("ln_b"), scalar1=cfg.alpha, scalar2=None, op0=OP.mult),
             reads=["cst"], writes=["lnb"])

        for kc in range(KC):
            P.op("sp", lambda e, kc=kc: e.dma_start(out=S[:, kc, :], in_=xT[kc * 128:(kc + 1) * 128, :]),
                 writes=[("S", kc, t) for t in range(NT)], kind="d", semkey="Sld%d" % kc)
        for kc in range(KC):
            for t in range(NT):
                P.op("pool", lambda e, kc=kc, t=t: e.tensor_scalar(out=S[:, kc, t * TT:(t + 1) * TT], in0=S[:, kc, t * TT:(t + 1) * TT],
                                                                    scalar1=cfg.alpha, scalar2=None, op0=OP.mult),
                     reads=[("S", kc, t)], writes=[("S", kc, t)])

        hbuf = [sb("h%d" % i, [128, KC, TT], BF16) for i in range(2)]
        rbuf = sb("rbuf", [128, KC, TT], F32)
        ybuf = sb("ybuf", [128, KC, TT], F32)
        sqb = ybuf
        stat = sb("stat", [128, 3, TT], F32)
        hcnt = [0]

        def tile_batch(t):
            return (t * TT) // L

        def make_h(l, sub, t, tt=None, tsz=None):
            hi = hcnt[0] % 2
            hcnt[0] += 1
            b = tile_batch(t)
            for kc in range(KC):
                eng = "act" if kc % 2 == 0 else "dve"
                shcol = modcol(l, 3 * sub, kc, b)
                m1 = dsc[:, l, sub, 0, kc, b:b + 1]
                if eng == "act":
                    P.op("act", lambda e, hi=hi, kc=kc, t=t, m1=m1, shcol=shcol: e.activation(
                        out=hbuf[hi][:, kc, :], in_=S[:, kc, t * TT:(t + 1) * TT], func=AF.Identity, scale=m1, bias=shcol),
                        reads=[("S", kc, t), "dsc", "modsb"], writes=[("h", hi, kc)])
                else:
                    P.op("dve", lambda e, hi=hi, kc=kc, t=t, m1=m1, shcol=shcol: e.tensor_scalar(
                        out=hbuf[hi][:, kc, :], in0=S[:, kc, t * TT:(t + 1) * TT], scalar1=m1, scalar2=shcol, op0=OP.mult, op1=OP.add),
                        reads=[("S", kc, t), "dsc", "modsb"], writes=[("h", hi, kc)])
            return hi

        def residual_ln(l, sub, t, ysrc):
            b = tile_batch(t)
            for kc in range(KC):
                yap, yres = ysrc(kc)
                gcol = dsc[:, l, sub, 1, kc, b:b + 1]
                P.op("dve", lambda e, kc=kc, yap=yap, gcol=gcol, t=t: e.scalar_tensor_tensor(
                    out=rbuf[:, kc, :], in0=yap, scalar=gcol, in1=S[:, kc, t * TT:(t + 1) * TT], op0=OP.mult, op1=OP.add),
                    reads=[yres, ("S", kc, t), "dsc"], writes=[("r", kc)])
                P.op("act", lambda e, kc=kc: e.activation(out=sqb[:, kc, :], in_=rbuf[:, kc, :], func=AF.Square),
                     reads=[("r", kc)], writes=[("y", kc)])
            p1, p2 = next_ps(), next_ps()
            for kc in range(KC):
                P.op("pe", lambda e, kc=kc, p1=p1: e.matmul(psum[p1][:, :TT], ones_f[:], rbuf[:, kc, :], start=(kc == 0), stop=(kc == KC - 1)),
                     reads=[("r", kc), "ones_f"], writes=["ps%d" % p1])
            for kc in range(KC):
                P.op("pe", lambda e, kc=kc, p2=p2: e.matmul(psum[p2][:, :TT], ones_f[:], sqb[:, kc, :], start=(kc == 0), stop=(kc == KC - 1)),
                     reads=[("y", kc), "ones_f"], writes=["ps%d" % p2])
            P.op("act", lambda e, p1=p1: e.activation(out=stat[:, 0, :], in_=psum[p1][:, :TT], func=AF.Copy), reads=["ps%d" % p1], writes=["stat0"])
            P.op("dve", lambda e: e.tensor_tensor(out=stat[:, 2, :], in0=stat[:, 0, :], in1=stat[:, 0, :], op=OP.mult), reads=["stat0"], writes=["stat2"])
            P.op("dve", lambda e, p2=p2: e.tensor_tensor(out=stat[:, 1, :], in0=psum[p2][:, :TT], in1=stat[:, 2, :], op=OP.subtract),
                 reads=["ps%d" % p2, "stat2"], writes=["stat1"])
            P.op("act", lambda e: e.activation(out=stat[:, 1, :], in_=stat[:, 1, :], func=AF.Ln, bias=C("eps_col")), reads=["stat1", "cst"], writes=["stat1"])
            P.op("act", lambda e: e.activation(out=stat[:, 1, :], in_=stat[:, 1, :], func=AF.Exp, scale=-0.5), reads=["stat1"], writes=["stat1"])
            li = (l * 2 + sub) * KC
            for kc in range(KC):
                P.op("dve", lambda e, kc=kc: e.tensor_tensor(out=rbuf[:, kc, :], in0=rbuf[:, kc, :], in1=stat[:, 0, :], op=OP.subtract),
                     reads=[("r", kc), "stat0"], writes=[("r", kc)])
                P.op("pool", lambda e, kc=kc: e.tensor_tensor(out=rbuf[:, kc, :], in0=rbuf[:, kc, :], in1=stat[:, 1, :], op=OP.mult),
                     reads=[("r", kc), "stat1"], writes=[("r", kc)])
                P.op("act", lambda e, kc=kc, t=t, li=li: e.activation(out=S[:, kc, t * TT:(t + 1) * TT], in_=rbuf[:, kc, :], func=AF.Identity,
                                                                      scale=lng[:, li + kc:li + kc + 1], bias=lnb[:, li + kc:li + kc + 1]),
                     reads=[("r", kc), "lng", "lnb"], writes=[("S", kc, t)])

        FB = {}

        def ffn_alloc():
            FB["w13r"] = [ar("w13r%d" % i, [128, 2 * KC * 128], BF16) for i in range(3)]
            FB["w2r"] = [ar("w2r%d" % i, [128, FC * 128], BF16) for i in range(2)]
            FB["Gt"] = ar("Gt", [128, FC, TT], BF16)
            FB["sil"] = [ar("sil%d" % i, [128, TT], F32) for i in range(2)]
        ring_cnt = {"w13": 0, "w2": 0, "sil": 0}

        def ffn_tile(tag, hi, yout):
            w13r, w2r, Gt, sil = FB["w13r"], FB["w2r"], FB["Gt"], FB["sil"]
            for fc in range(FC):
                s = ring_cnt["w13"] % 3
                ring_cnt["w13"] += 1
                src, res, free = wblock(tag + "w13", fc)
                P.op("sp", lambda e, s=s, src=src: e.dma_start(out=w13r[s][:], in_=src), reads=[res], writes=["w13r%d" % s], kind="d")
                pa, pb = next_ps(), next_ps()
                for half, pi in ((0, pa), (1, pb)):
                    for kc in range(KC):
                        o = (half * KC + kc) * 128
                        P.op("pe", lambda e, s=s, o=o, pi=pi, kc=kc: e.matmul(psum[pi][:, :TT], w13r[s][:, o:o + 128], hbuf[hi][:, kc, :],
                                                                              start=(kc == 0), stop=(kc == KC - 1)),
                             reads=["w13r%d" % s, ("h", hi, kc)], writes=["ps%d" % pi])
                si = ring_cnt["sil"] % 2
                ring_cnt["sil"] += 1
                P.op("act", lambda e, si=si, pa=pa: e.activation(out=sil[si][:], in_=psum[pa][:, :TT], func=AF.Silu),
                     reads=["ps%d" % pa], writes=["sil%d" % si])
                P.op("dve", lambda e, si=si, pb=pb, fc=fc: e.tensor_tensor(out=Gt[:, fc, :], in0=sil[si][:], in1=psum[pb][:, :TT], op=OP.mult),
                     reads=["sil%d" % si, "ps%d" % pb], writes=[("G", fc)])
            for dc in range(KC):
                s = ring_cnt["w2"] % 2
                ring_cnt["w2"] += 1
                src, res, free = wblock(tag + "w2", dc)
                P.op("sp", lambda e, s=s, src=src: e.dma_start(out=w2r[s][:], in_=src), reads=[res], writes=["w2r%d" % s], kind="d")
                py = next_ps()
                for fc in range(FC):
                    P.op("pe", lambda e, s=s, fc=fc, py=py: e.matmul(psum[py][:, :TT], w2r[s][:, fc * 128:(fc + 1) * 128], Gt[:, fc, :],
                                                                     start=(fc == 0), stop=(fc == FC - 1)),
                         reads=["w2r%d" % s, ("G", fc)], writes=["ps%d" % py])
                yout(dc, py)


        def dense_ffn_layer(l, i):
            areset()
            ffn_alloc()
            for t in range(NT):
                hi = make_h(l, 1, t)

                def yout(dc, py):
                    P.op("act", lambda e, dc=dc, py=py: e.activation(out=ybuf[:, dc, :], in_=psum[py][:, :TT], func=AF.Copy),
                         reads=["ps%d" % py], writes=[("y", dc)])
                ffn_tile("ff%d" % i, hi, yout)
                residual_ln(l, 1, t, lambda kc: (ybuf[:, kc, :], ("y", kc)))

        def moe_layer(l, m):
            areset()
            ffn_alloc()
            gwT = ar("gwT%d" % m, [128, TT], F32)
            lg = ar("lg%d" % m, [128, 128], F32)
            gw = ar("gw%d" % m, [128, 128], F32)
            mx = ar("mx%d" % m, [128, 8], F32)
            tmp1 = ar("mtmp%d" % m, [128, 8], F32)
            gwb = ar("gwb%d" % m, [128, TT], F32)
            wrs = ar("wrs%d" % m, [128, KC * 128], BF16)
            selt = ar("selt%d" % m, [128, E * 128], F32)
            load_big("sel", selt[:], "selt")
            ident_f = C("ident")
            bro, _ = cidx["moe_br"]
            src, res, free = wblock("mo%dwr" % m, 0)
            P.op("sp", lambda e, src=src: e.dma_start(out=wrs[:], in_=src), reads=[res], writes=["wrs"], kind="d")
            for t in range(NT):
                hi = make_h(l, 1, t)
                for tb in range(TT // 128):
                    pl = next_ps()
                    for kc in range(KC):
                        P.op("pe", lambda e, kc=kc, tb=tb, pl=pl, hi=hi: e.matmul(psum[pl][:, :128], hbuf[hi][:, kc, tb * 128:(tb + 1) * 128],
                                                                            wrs[:, kc * 128:(kc + 1) * 128], start=(kc == 0), stop=(kc == KC - 1)),
                             reads=[("h", hi, kc), "wrs"], writes=["ps%d" % pl])
                    P.op("dve", lambda e, pl=pl: e.tensor_tensor(out=lg[:, :E], in0=psum[pl][:, :E], in1=cst[:, bro + m * E: bro + (m + 1) * E], op=OP.add),
                         reads=["ps%d" % pl, "cst"], writes=["lg"])
                    P.op("dve", lambda e: e.max(out=mx[:], in_=lg[:, :E]), reads=["lg"], writes=["mx"])
                    P.op("dve", lambda e: e.tensor_scalar(out=gw[:, :E], in0=lg[:, :E], scalar1=mx[:, 0:1], scalar2=None, op0=OP.subtract),
                         reads=["lg", "mx"], writes=["gw"])
                    P.op("act", lambda e: e.activation(out=gw[:, :E], in_=gw[:, :E], func=AF.Exp), reads=["gw"], writes=["gw"])
                    P.op("dve", lambda e: e.tensor_tensor(out=tmp1[:, 0:1], in0=mx[:, 1:2], in1=mx[:, 0:1], op=OP.subtract), reads=["mx"], writes=["tmp1"])
                    P.op("act", lambda e: e.activation(out=tmp1[:, 1:2], in_=tmp1[:, 0:1], func=AF.Exp), reads=["tmp1"], writes=["tmp1b"])
                    P.op("dve", lambda e: e.tensor_scalar(out=tmp1[:, 2:3], in0=tmp1[:, 1:2], scalar1=1.0, scalar2=None, op0=OP.add), reads=["tmp1b"], writes=["tmp1c"])
                    P.op("dve", lambda e: e.reciprocal(out=tmp1[:, 3:4], in_=tmp1[:, 2:3]), reads=["tmp1c"], writes=["tmp1d"])
                    P.op("dve", lambda e: e.tensor_scalar(out=lg[:, :E], in0=lg[:, :E], scalar1=mx[:, 1:2], scalar2=tmp1[:, 3:4], op0=OP.is_ge, op1=OP.mult),
                         reads=["lg", "mx", "tmp1d"], writes=["lg"])
                    P.op("dve", lambda e: e.tensor_tensor(out=gw[:, :E], in0=gw[:, :E], in1=lg[:, :E], op=OP.mult), reads=["gw", "lg"], writes=["gw"])
                    pt = next_ps()
                    P.op("pe", lambda e, pt=pt: e.transpose(psum[pt][:E, :128], gw[:, :E], ident_f), reads=["gw", "cst"], writes=["ps%d" % pt])
                    P.op("act", lambda e, pt=pt, tb=tb: e.activation(out=gwT[:E, tb * 128:(tb + 1) * 128], in_=psum[pt][:E, :128], func=AF.Copy),
                         reads=["ps%d" % pt], writes=["gwT"])
                first = [True] * KC
                for ex in range(E):
                    pg = next_ps()
                    P.op("pe", lambda e, ex=ex, pg=pg: e.matmul(psum[pg][:, :TT], selt[:E, ex * 128:(ex + 1) * 128], gwT[:E, :],
                                                                 start=True, stop=True), reads=["gwT", "selt"], writes=["ps%d" % pg])
                    P.op("act", lambda e, pg=pg: e.activation(out=gwb[:], in_=psum[pg][:, :TT], func=AF.Copy), reads=["ps%d" % pg], writes=["gwb"])

                    def yout(dc, py, ex=ex):
                        if ex == 0:
                            P.op("dve", lambda e, dc=dc, py=py: e.tensor_tensor(out=ybuf[:, dc, :], in0=psum[py][:, :TT], in1=gwb[:], op=OP.mult),
                                 reads=["ps%d" % py, "gwb"], writes=[("y", dc)])
                        else:
                            P.op("dve", lambda e, dc=dc, py=py: e.tensor_tensor(out=rbuf[:, dc, :], in0=psum[py][:, :TT], in1=gwb[:], op=OP.mult),
                                 reads=["ps%d" % py, "gwb"], writes=[("r", dc)])
                            P.op("pool", lambda e, dc=dc: e.tensor_tensor(out=ybuf[:, dc, :], in0=ybuf[:, dc, :], in1=rbuf[:, dc, :], op=OP.add),
                                 reads=[("r", dc), ("y", dc)], writes=[("y", dc)])
                    ffn_tile("mo%de%d" % (m, ex), hi, yout)
                residual_ln(l, 1, t, lambda kc: (ybuf[:, kc, :], ("y", kc)))

        wstr = []
        wstr_cnt = [0]

        def wstr_alloc():
            wstr.clear()
            wstr.extend([ar("wstr%d" % i, [128, max(KC, cfg.GC) * 128], BF16) for i in range(2)])

        def load_stat(name, kblk):
            s = wstr_cnt[0] % 2
            wstr_cnt[0] += 1
            src, res, free = wblock(name, kblk)
            P.op("sp", lambda e, dst=wstr[s], src=src, free=free: e.dma_start(out=dst[:, :free], in_=src), reads=[res], writes=["wstr%d" % s], kind="d")
            return s

        def proj_fm(name, mc, rhs_fn, nk, ntok, pi):
            s = load_stat(name, mc)
            for kk in range(nk):
                rap, rres = rhs_fn(kk)
                P.op("pe", lambda e, wt=wstr[s], kk=kk, rap=rap, pi=pi: e.matmul(psum[pi][:, :ntok], wt[:, kk * 128:(kk + 1) * 128], rap,
                                                                           start=(kk == 0), stop=(kk == nk - 1)),
                     reads=["wstr%d" % s, rres], writes=["ps%d" % pi])

        def gmlp_layer(l, j):
            TG = cfg.TTG
            GC = cfg.GC
            gd_chunks = GC // cfg.GMG
            areset()
            wstr_alloc()
            wsb = ar("gm_ws%d" % j, [128, cfg.GMG * 128], BF16)
            MB = ar("gm_MB%d" % j, [128, GC, 128], F32)
            wv = [ar("gm_wv%d_%d" % (j, i), [128, KC * 512], BF16) for i in range(1)]
            u = ar("gm_u%d" % j, [128, GC, TG], BF16)
            um = ar("gm_um%d" % j, [128, GC, TG], BF16)
            vpre = ar("gm_vpre%d" % j, [128, TG // 128, cfg.GMW], F32)
            vhat = [ar("gm_vhat%d_%d" % (j, i), [128, cfg.GMW], BF16) for i in range(2)]
            bst = ar("gm_bst%d" % j, [128, cfg.GMW // 512 + 1, 6], F32)
            mv = ar("gm_mv%d" % j, [128, 8], F32)
            mixed = ar("gm_mixed%d" % j, [128, 128], F32)
            bsT = ar("gm_bsT%d" % j, [128, cfg.GMG * 128], F32)
            ob_, nb_ = cidx["gm_bs"]
            P.op("sp", lambda e: e.dma_start(out=bsT[:], in_=cst_d[:, ob_ + j * cfg.GMG * 128: ob_ + (j + 1) * cfg.GMG * 128]), writes=["gm_bsT"], kind="d")
            trio, _ = cidx["tri"]
            glg, _ = cidx["gm_ln_g"]
            glb, _ = cidx["gm_ln_b"]
            src, res, free = wblock("gm%dws" % j, 0)
            P.op("sp", lambda e, src=src: e.dma_start(out=wsb[:], in_=src), reads=[res], writes=["gm_ws"], kind="d")
            for g in range(cfg.GMG):
                P.op("dve", lambda e, g=g: e.tensor_tensor(out=wsb[:, g * 128:(g + 1) * 128], in0=wsb[:, g * 128:(g + 1) * 128],
                                                            in1=cst[:, trio:trio + 128], op=OP.mult), reads=["gm_ws", "cst"], writes=["gm_ws"])
            for g in range(cfg.GMG):
                pr = next_ps()
                P.op("pe", lambda e, g=g, pr=pr: e.matmul(psum[pr][:, :128], ones_b[:], wsb[:, g * 128:(g + 1) * 128], start=True, stop=True),
                     reads=["gm_ws", "ones_b"], writes=["ps%d" % pr])
                for cc in range(gd_chunks):
                    fc = g * gd_chunks + cc
                    P.op("dve", lambda e, g=g, fc=fc, pr=pr: e.scalar_tensor_tensor(
                        out=MB[:, fc, :], in0=psum[pr][:, :128], scalar=cst[:, glb + j * GC + fc: glb + j * GC + fc + 1],
                        in1=bsT[:, g * 128:(g + 1) * 128], op0=OP.mult, op1=OP.add),
                        reads=["ps%d" % pr, "cst", "gm_bsT"], writes=["gm_MB"])
            NTG = T // TG
            for t in range(NTG):
                b = (t * TG) // L
                hi = hcnt[0] % 2
                hcnt[0] += 1
                for kc in range(KC):
                    shcol = modcol(l, 0, kc, b)
                    m1 = dsc[:, l, 0, 0, kc, b:b + 1]
                    P.op("act", lambda e, hi=hi, kc=kc, t=t, m1=m1, shcol=shcol: e.activation(
                        out=hbuf[hi][:, kc, :TG], in_=S[:, kc, t * TG:(t + 1) * TG], func=AF.Identity, scale=m1, bias=shcol),
                        reads=[("S", kc, (t * TG) // TT), "dsc", "modsb"], writes=[("h", hi, kc)])
                for mc in range(GC):
                    pu = next_ps()
                    proj_fm("gm%dwu" % j, mc, lambda kk: (hbuf[hi][:, kk, :TG], ("h", hi, kk)), KC, TG, pu)
                    P.op("act", lambda e, mc=mc, pu=pu: e.activation(out=u[:, mc, :], in_=psum[pu][:, :TG], func=AF.Gelu),
                         reads=["ps%d" % pu], writes=[("gm_u", mc)])
                for cg in range(cfg.GMW // 512):
                    s = 0
                    src, res, free = wblock("gm%dwv" % j, cg)
                    P.op("sp", lambda e, s=s, src=src: e.dma_start(out=wv[s][:], in_=src), reads=[res], writes=["gm_wv%d" % s], kind="d")
                    for tb in range(TG // 128):
                        pv = next_ps()
                        for kc in range(KC):
                            P.op("pe", lambda e, s=s, kc=kc, tb=tb, pv=pv, hi=hi: e.matmul(psum[pv][:, :512], hbuf[hi][:, kc, tb * 128:(tb + 1) * 128],
                                                                                    wv[s][:, kc * 512:(kc + 1) * 512], start=(kc == 0), stop=(kc == KC - 1)),
                                 reads=["gm_wv%d" % s, ("h", hi, kc)], writes=["ps%d" % pv])
                        P.op("act", lambda e, tb=tb, cg=cg, pv=pv: e.activation(out=vpre[:, tb, cg * 512:(cg + 1) * 512], in_=psum[pv][:, :512], func=AF.Gelu),
                             reads=["ps%d" % pv], writes=[("gm_vpre", tb)])
                for tb in range(TG // 128):
                    ncg = cfg.GMW // 512
                    for cg in range(ncg):
                        P.op("dve", lambda e, tb=tb, cg=cg: e.bn_stats(out=bst[:, cg, :], in_=vpre[:, tb, cg * 512:(cg + 1) * 512]),
                             reads=[("gm_vpre", tb)], writes=["gm_bst"])
                    P.op("dve", lambda e, ncg=ncg: e.bn_aggr(out=mv[:, 0:2], in_=bst[:, :ncg, :].rearrange("p a b -> p (a b)")),
                         reads=["gm_bst"], writes=["gm_mv"])
                    P.op("act", lambda e: e.activation(out=mv[:, 2:3], in_=mv[:, 1:2], func=AF.Ln, bias=C("eps_col")), reads=["gm_mv", "cst"], writes=["gm_mv2"])
                    P.op("act", lambda e: e.activation(out=mv[:, 2:3], in_=mv[:, 2:3], func=AF.Exp, scale=-0.5), reads=["gm_mv2"], writes=["gm_mv2"])
                    vh = tb % 2
                    P.op("dve", lambda e, tb=tb, vh=vh: e.tensor_scalar(out=vhat[vh][:], in0=vpre[:, tb, :], scalar1=mv[:, 0:1], scalar2=mv[:, 2:3],
                                                                          op0=OP.subtract, op1=OP.mult),
                         reads=[("gm_vpre", tb), "gm_mv", "gm_mv2"], writes=["gm_vhat%d" % vh])
                    for fc in range(GC):
                        g = fc // gd_chunks
                        pm = next_ps()
                        P.op("pe", lambda e, vh=vh, fc=fc, g=g, pm=pm: e.matmul(psum[pm][:, :128], vhat[vh][:, fc * 128:(fc + 1) * 128],
                                                                                 wsb[:, g * 128:(g + 1) * 128], start=True, stop=True),
                             reads=["gm_vhat%d" % vh, "gm_ws"], writes=["ps%d" % pm])
                        P.op("dve", lambda e, fc=fc, pm=pm: e.scalar_tensor_tensor(
                            out=mixed[:], in0=psum[pm][:, :128], scalar=cst[:, glg + j * GC + fc: glg + j * GC + fc + 1], in1=MB[:, fc, :],
                            op0=OP.mult, op1=OP.add), reads=["ps%d" % pm, "gm_MB", "cst"], writes=["gm_mixed"])
                        P.op("pool", lambda e, fc=fc, tb=tb: e.tensor_tensor(out=um[:, fc, tb * 128:(tb + 1) * 128], in0=mixed[:],
                                                                              in1=u[:, fc, tb * 128:(tb + 1) * 128], op=OP.mult),
                             reads=["gm_mixed", ("gm_u", fc)], writes=[("gm_um", fc)])
                tt_t = (t * TG) // TT
                col0 = (t * TG) % TT
                for dc in range(KC):
                    py = next_ps()
                    proj_fm("gm%dwo" % j, dc, lambda kk: (um[:, kk, :], ("gm_um", kk)), GC, TG, py)
                    P.op("act", lambda e, dc=dc, py=py, col0=col0: e.activation(out=ybuf[:, dc, col0:col0 + TG], in_=psum[py][:, :TG], func=AF.Copy),
                         reads=["ps%d" % py], writes=[("y", dc)])
                if col0 + TG == TT:
                    residual_ln(l, 0, tt_t, lambda kc: (ybuf[:, kc, :], ("y", kc)))

        def pool_layer(l, j):
            areset()
            wstr_alloc()
            zb = ar("pl_z%d" % j, [128, KC, 16 + L], F32)
            wk = ar("pl_wk%d" % j, [128, 16 + L], F32)
            wk2 = ar("pl_wk2%d" % j, [128, 16 + L], F32)
            pooled = ar("pl_pooled%d" % j, [128, KC, TT], BF16)
            y1 = ar("pl_y1%d" % j, [128, KC, TT], BF16)
            hh = ar("pl_hh%d" % j, [128, KC, 16], BF16)
            halo_all = ar("pl_ha%d" % j, [128, NCORES, KC * 2 * 16], F32)
            halo_sel = ar("pl_hs%d" % j, [128, KC, 2, 16], F32)
            halo_loc = ar("pl_hl%d" % j, [128, KC, 2, 16], F32)
            ptmp = ar("pl_tmp%d" % j, [128, 16], F32)
            nfo, _ = cidx["not_first"]
            hdl = nc.dram_tensor("pl_hdl%d" % j, [128, KC * 2 * 16], F32).ap()
            hda = nc.dram_tensor("pl_hda%d" % j, [NCORES * 128, KC * 2 * 16], F32).ap()
            pso, _ = cidx["pool_scale"]
            rco, _ = cidx["pool_rc"]
            pho, _ = cidx["prev_onehot"]
            gd_chunks = KC // cfg.PG
            for kc in range(KC):
                for seg in range(2):
                    t_last = (seg * L + L - 16) // TT
                    P.op("pool", lambda e, kc=kc, seg=seg: e.tensor_copy(out=halo_loc[:, kc, seg, :], in_=S[:, kc, seg * L + L - 16: seg * L + L]),
                         reads=[("S", kc, t_last)], writes=["pl_hl"])
            P.op("act", lambda e: e.dma_start(out=hdl, in_=halo_loc[:].rearrange("p a b c -> p (a b c)")), reads=["pl_hl"], writes=["pl_hdl"], kind="d")
            P.op("pool", lambda e: e.collective_compute("AllGather", OP.bypass, replica_groups=groups, ins=[hdl], outs=[hda]),
                 reads=["pl_hdl"], writes=["pl_hda"], kind="cc")
            P.op("sp", lambda e: e.dma_start(out=halo_all[:], in_=hda.rearrange("(r p) f -> p r f", p=128)), reads=["pl_hda"], writes=["pl_ha"], kind="d")
            hs_flat = halo_sel[:].rearrange("p a b c -> p (a b c)")
            P.op("dve", lambda e: e.tensor_scalar(out=hs_flat, in0=halo_all[:, 0, :], scalar1=cst[:, pho:pho + 1], scalar2=None, op0=OP.mult),
                 reads=["pl_ha", "cst"], writes=["pl_hs"])
            for r in range(1, NCORES):
                P.op("dve", lambda e, r=r: e.scalar_tensor_tensor(out=hs_flat, in0=halo_all[:, r, :], scalar=cst[:, pho + r:pho + r + 1], in1=hs_flat,
                                                                   op0=OP.mult, op1=OP.add), reads=["pl_ha", "cst", "pl_hs"], writes=["pl_hs"])
            for seg in range(2):
                b = seg
                for kc in range(KC):
                    shcol = modcol(l, 0, kc, b)
                    m1 = dsc[:, l, 0, 0, kc, b:b + 1]
                    P.op("dve", lambda e, kc=kc, seg=seg, m1=m1, shcol=shcol: e.tensor_scalar(
                        out=hh[:, kc, :], in0=halo_sel[:, kc, seg, :], scalar1=m1, scalar2=shcol, op0=OP.mult, op1=OP.add),
                        reads=["pl_hs", "dsc", "modsb"], writes=["pl_hh"])
                for mc in range(KC):
                    pz = next_ps()
                    proj_fm("pl%dwi" % j, mc, lambda kk: (hh[:, kk, :], "pl_hh"), KC, 16, pz)
                    P.op("act", lambda e, mc=mc, pz=pz: e.activation(out=zb[:, mc, 0:16], in_=psum[pz][:, :16], func=AF.Copy, scale=cst[:, nfo:nfo + 1]),
                         reads=["ps%d" % pz, "cst"], writes=[("pl_z", mc)])
                for tl in range(L // TT):
                    t = seg * (L // TT) + tl
                    hi = make_h(l, 0, t)
                    for mc in range(KC):
                        pz = next_ps()
                        proj_fm("pl%dwi" % j, mc, lambda kk: (hbuf[hi][:, kk, :], ("h", hi, kk)), KC, TT, pz)
                        P.op("act", lambda e, mc=mc, pz=pz, tl=tl: e.activation(out=zb[:, mc, 16 + tl * TT: 16 + (tl + 1) * TT], in_=psum[pz][:, :TT], func=AF.Copy),
                             reads=["ps%d" % pz], writes=[("pl_z", mc)])
                for tl in range(L // TT):
                    t = seg * (L // TT) + tl
                    for mc in range(KC):
                        g = mc // gd_chunks
                        nsteps = g + 1
                        win = 2 ** nsteps
                        cur_ap = zb[:, mc, :]
                        bufs = [wk, wk2]
                        lo = 16 + tl * TT - 16
                        hi_ = 16 + (tl + 1) * TT
                        for st in range(nsteps):
                            sh = 2 ** st
                            dst = bufs[st % 2]
                            a0 = lo + 2 * sh - 1
                            P.op("dve" if (mc % 2 == 0) else "pool", lambda e, cur_ap=cur_ap, dst=dst, a0=a0, hi_=hi_, sh=sh: e.tensor_tensor(
                                out=dst[:, a0:hi_], in0=cur_ap[:, a0:hi_], in1=cur_ap[:, a0 - sh:hi_ - sh], op=OP.add),
                                reads=[("pl_z", mc), "pl_wk", "pl_wk2"], writes=["pl_wk" if st % 2 == 0 else "pl_wk2"])
                            cur_ap = dst[:]
                        c0 = 16 + tl * TT
                        P.op("dve", lambda e, cur_ap=cur_ap, mc=mc, c0=c0, win=win: e.scalar_tensor_tensor(
                            out=pooled[:, mc, :], in0=cur_ap[:, c0:c0 + TT], scalar=1.0 / win, in1=zb[:, mc, c0:c0 + TT], op0=OP.mult, op1=OP.subtract),
                            reads=["pl_wk", "pl_wk2", ("pl_z", mc)], writes=[("pl_pooled", mc)])
                        if tl == 0:
                            P.op("dve", lambda e, cur_ap=cur_ap, mc=mc, g=g: e.tensor_tensor(
                                out=ptmp[:, 0:16], in0=cur_ap[:, 16:32], in1=cst[:, rco + g * 16: rco + (g + 1) * 16], op=OP.mult),
                                reads=["pl_wk", "pl_wk2", "cst"], writes=["sil0"])
                            P.op("dve", lambda e, mc=mc: e.tensor_tensor(out=pooled[:, mc, 0:16], in0=ptmp[:, 0:16], in1=zb[:, mc, 16:32], op=OP.subtract),
                                 reads=["sil0", ("pl_z", mc)], writes=[("pl_pooled", mc)])
                    for g in range(cfg.PG):
                        for dcg in range(gd_chunks):
                            mc = g * gd_chunks + dcg
                            pg_ = next_ps()
                            proj_fm("pl%dwg" % j, mc, lambda kk, g=g: (pooled[:, g * gd_chunks + kk, :], ("pl_pooled", g * gd_chunks + kk)), gd_chunks, TT, pg_)
                            P.op("act", lambda e, mc=mc, pg_=pg_: e.activation(out=y1[:, mc, :], in_=psum[pg_][:, :TT], func=AF.Copy,
                                                                               scale=cst[:, pso + j * KC + mc: pso + j * KC + mc + 1]),
                                 reads=["ps%d" % pg_, "cst"], writes=[("pl_y1", mc)])
                    for dc in range(KC):
                        py = next_ps()
                        proj_fm("pl%dwo" % j, dc, lambda kk: (y1[:, kk, :], ("pl_y1", kk)), KC, TT, py)
                        P.op("act", lambda e, dc=dc, py=py: e.activation(out=ybuf[:, dc, :], in_=psum[py][:, :TT], func=AF.Copy),
                             reads=["ps%d" % py], writes=[("y", dc)])
                    residual_ln(l, 0, t, lambda kc: (ybuf[:, kc, :], ("y", kc)))

        def fox_layer(l, j):
            H, HD = cfg.H, cfg.HD
            NB = L // 128
            NQ = L // TT
            NBT = T // 128
            KA = min(4, KC)
            n_ag = KC // KA
            qscale = HD ** -0.5
            qd = nc.dram_tensor("fx_q%d" % j, [D, T], BF16).ap()

            def agpair(name, rows, cols):
                nw = rows * cols
                cc = None
                for c_ in (2048, 1024, 512, 256, 128, 64, 32):
                    if nw % c_ == 0 and nw // c_ <= 256:
                        cc = c_
                        break
                assert cc is not None, (name, rows, cols)
                rr = nw // cc
                i_raw = nc.dram_tensor(name + "_l", [rr, cc], F32).ap()
                o_raw = nc.dram_tensor(name + "_a", [NCORES * rr, cc], F32).ap()
                i_v = i_raw.rearrange("a b -> (a b)").rearrange("(x y) -> x y", y=cols)
                o_v = o_raw.rearrange("a b -> (a b)").rearrange("(x y) -> x y", y=cols)
                return i_raw, o_raw, i_v, o_v
            kp = [agpair("fx_k%d_%d" % (j, a), KA * 128, T // 2) for a in range(n_ag)]
            vp = [agpair("fx_v%d_%d" % (j, s_), L, D // 2) for s_ in range(2)]
            cp_ = agpair("fx_c%d" % j, 128, T)
            kloc_raw, kall_raw = [p_[0] for p_ in kp], [p_[1] for p_ in kp]
            kloc, kall = [p_[2] for p_ in kp], [p_[3] for p_ in kp]
            vloc_raw, vall_raw = [p_[0] for p_ in vp], [p_[1] for p_ in vp]
            vloc, vall = [p_[2] for p_ in vp], [p_[3] for p_ in vp]
            cloc_raw, call_raw, cloc, call = cp_
            cqd = nc.dram_tensor("fx_cq%d" % j, [128, 3 * T], BF16).ap()
            nbd = nc.dram_tensor("fx_nb%d" % j, [128, NCORES * NBT * H], F32).ap()
            atd = nc.dram_tensor("fx_at%d" % j, [D, T], BF16).ap()
            kloc_bf = [k_.bitcast(BF16) for k_ in kloc]
            kall_bf = [k_.bitcast(BF16) for k_ in kall]
            vloc_bf = [v_.bitcast(BF16) for v_ in vloc]
            vall_bf = [v_.bitcast(BF16) for v_ in vall]
            bfo, _ = cidx["fox_bf"]
            oco, _ = cidx["one_col"]
            mlt, _ = cidx["m_lt"]
            mgt, _ = cidx["m_gtbig"]
            meq, _ = cidx["m_eq"]
            mne, _ = cidx["m_neq"]
            nbe, _ = cidx["negbig_eq"]
            areset()
            wstr_alloc()
            qst = ar("fx_qst", [128, KC, TT], BF16)
            kst = ar("fx_kst", [128, KC, TT], BF16)
            vst = ar("fx_vst", [128, TT // 128, D], BF16)
            wvf = ar("fx_wvf", [128, KC * 512], BF16)
            fa = ar("fx_fa", [128, TT], F32)
            fe = ar("fx_fe", [128, TT], F32)
            fl = ar("fx_fl", [128, TT], F32)
            ones_t = ar("fx_ones", [128, TT], F32)
            cumT = ar("fx_cumT", [128, T], F32)
            P.op("pool", lambda e: e.memset(ones_t[:], 1.0), writes=["fx_ones"])
            for t in range(NT):
                seg = (t * TT) // L
                tl = t - seg * NQ
                hi = make_h(l, 0, t)
                for mc in range(KC):
                    pq = next_ps()
                    proj_fm("fx%dwq" % j, mc, lambda kk: (hbuf[hi][:, kk, :], ("h", hi, kk)), KC, TT, pq)
                    P.op("act", lambda e, mc=mc, pq=pq: e.activation(out=qst[:, mc, :], in_=psum[pq][:, :TT], func=AF.Copy, scale=qscale),
                         reads=["ps%d" % pq], writes=["fx_qst"])
                    pk_ = next_ps()
                    proj_fm("fx%dwk" % j, mc, lambda kk: (hbuf[hi][:, kk, :], ("h", hi, kk)), KC, TT, pk_)
                    P.op("dve", lambda e, mc=mc, pk_=pk_: e.tensor_copy(out=kst[:, mc, :], in_=psum[pk_][:, :TT]),
                         reads=["ps%d" % pk_], writes=["fx_kst"])
                P.op("act", lambda e, t=t: e.dma_start(out=qd.rearrange("(kc p) t -> p kc t", p=128)[:, :, t * TT:(t + 1) * TT], in_=qst[:]),
                     reads=["fx_qst"], writes=["fx_q"], kind="d")
                for a in range(n_ag):
                    P.op("act", lambda e, t=t, a=a: e.dma_start(out=kloc_bf[a].rearrange("(kc p) t -> p kc t", p=128)[:, :, t * TT:(t + 1) * TT],
                                                                   in_=kst[:, a * KA:(a + 1) * KA, :]),
                         reads=["fx_kst"], writes=["fx_kl%d" % a], kind="d")
                for cg in range(D // 512):
                    src, res, free = wblock("fx%dwv" % j, cg)
                    P.op("sp", lambda e, src=src: e.dma_start(out=wvf[:], in_=src), reads=[res], writes=["fx_wvf"], kind="d")
                    for tb in range(TT // 128):
                        pv = next_ps()
                        for kc in range(KC):
                            P.op("pe", lambda e, kc=kc, tb=tb, pv=pv, hi=hi: e.matmul(psum[pv][:, :512], hbuf[hi][:, kc, tb * 128:(tb + 1) * 128],
                                                                                      wvf[:, kc * 512:(kc + 1) * 512], start=(kc == 0), stop=(kc == KC - 1)),
                                 reads=["fx_wvf", ("h", hi, kc)], writes=["ps%d" % pv])
                        P.op("act", lambda e, tb=tb, cg=cg, pv=pv: e.activation(out=vst[:, tb, cg * 512:(cg + 1) * 512], in_=psum[pv][:, :512], func=AF.Copy),
                             reads=["ps%d" % pv], writes=["fx_vst"])
                P.op("act", lambda e, seg=seg, tl=tl: e.dma_start(
                    out=vloc_bf[seg][tl * TT:(tl + 1) * TT, :].rearrange("(tb p) d -> p tb d", p=128), in_=vst[:]),
                    reads=["fx_vst"], writes=["fx_vl%d" % seg], kind="d")
                pf = next_ps()
                proj_fm("fx%dwf" % j, 0, lambda kk: (hbuf[hi][:, kk, :], ("h", hi, kk)), KC, TT, pf)
                P.op("act", lambda e, pf=pf: e.activation(out=fa[:], in_=psum[pf][:, :TT], func=AF.Identity, bias=cst[:, bfo + j:bfo + j + 1]),
                     reads=["ps%d" % pf, "cst"], writes=["fx_fa"])
                P.op("act", lambda e: e.activation(out=fe[:], in_=fa[:], func=AF.Abs), reads=["fx_fa"], writes=["fx_fe"])
                P.op("act", lambda e: e.activation(out=fe[:], in_=fe[:], func=AF.Exp, scale=-1.0), reads=["fx_fe"], writes=["fx_fe"])
                P.op("act", lambda e: e.activation(out=fl[:], in_=fe[:], func=AF.Ln, bias=cst[:, oco:oco + 1]), reads=["fx_fe", "cst"], writes=["fx_fl"])
                P.op("dve", lambda e: e.scalar_tensor_tensor(out=fa[:], in0=fa[:], scalar=0.0, in1=fl[:], op0=OP.min, op1=OP.subtract),
                     reads=["fx_fa", "fx_fl"], writes=["fx_fa"])
                init = 0.0 if tl == 0 else cumT[:, t * TT - 1:t * TT]
                P.op("dve", lambda e, t=t, init=init: e.tensor_tensor_scan(out=cumT[:, t * TT:(t + 1) * TT], data0=ones_t[:], data1=fa[:], initial=init,
                                                                             op0=OP.mult, op1=OP.add),
                     reads=["fx_fa", "fx_ones", "fx_cumT"], writes=["fx_cumT"])
            P.op("act", lambda e: e.dma_start(out=cloc, in_=cumT[:]), reads=["fx_cumT"], writes=["fx_cl"], kind="d")
            for a in range(n_ag):
                P.op("pool", lambda e, a=a: e.collective_compute("AllGather", OP.bypass, replica_groups=groups, ins=[kloc_raw[a]], outs=[kall_raw[a]]),
                     reads=["fx_kl%d" % a], writes=["fx_ka%d" % a], kind="cc")
            for s_ in range(2):
                P.op("pool", lambda e, s_=s_: e.collective_compute("AllGather", OP.bypass, replica_groups=groups, ins=[vloc_raw[s_]], outs=[vall_raw[s_]]),
                     reads=["fx_vl%d" % s_], writes=["fx_va%d" % s_], kind="cc")
            P.op("pool", lambda e: e.collective_compute("AllGather", OP.bypass, replica_groups=groups, ins=[cloc_raw], outs=[call_raw]),
                 reads=["fx_cl"], writes=["fx_ca"], kind="cc")
            areset()
            crt = [ar("fx_crt%d" % i, [128, T], F32) for i in range(2)]
            nbT = ar("fx_nbT", [128, T], F32)
            nb = ar("fx_nb", [128, NCORES, NBT, H], F32)
            gsm = ar("fx_gsm", [128, 8], F32)
            cqs = ar("fx_cqs", [128, 3, T], BF16)
            cq32 = ar("fx_cq32", [128, T], F32)
            rem = ar("fx_rem", [128, T], F32)
            own = ar("fx_own", [128, T], F32)
            P.op("pool", lambda e: e.memset(gsm[:], 0.0), writes=["fx_gsm"])
            P.op("sp", lambda e: e.dma_start(out=own[:], in_=cloc), reads=["fx_cl"], writes=["fx_own"], kind="d")
            for r in range(NCORES):
                ci = r % 2
                P.op("sp", lambda e, r=r, ci=ci: e.dma_start(out=crt[ci][:], in_=call[r * 128:(r + 1) * 128, :]), reads=["fx_ca"], writes=["fx_crt%d" % ci], kind="d")
                for b in range(2):
                    P.op("dve", lambda e, r=r, b=b: e.tensor_tensor(out=gsm[:, 4 + b:5 + b], in0=gsm[:, b:b + 1], in1=cst[:, mgt + r:mgt + r + 1], op=OP.add),
                         reads=["fx_gsm", "cst"], writes=["fx_gsm"])
                    P.op("dve", lambda e, b=b, ci=ci: e.tensor_scalar(out=nbT[:, b * L:(b + 1) * L], in0=crt[ci][:, b * L:(b + 1) * L], scalar1=gsm[:, 4 + b:5 + b],
                                                                       scalar2=-1.0, op0=OP.add, op1=OP.mult),
                         reads=["fx_gsm", "fx_crt%d" % ci], writes=["fx_nbT"])
                for blk in range(NBT):
                    pt_ = next_ps()
                    P.op("pe", lambda e, blk=blk, pt_=pt_: e.transpose(psum[pt_][:, :128], nbT[:, blk * 128:(blk + 1) * 128], C("ident")),
                         reads=["fx_nbT", "cst"], writes=["ps%d" % pt_])
                    P.op("act", lambda e, r=r, blk=blk, pt_=pt_: e.activation(out=nb[:, r, blk, :], in_=psum[pt_][:, :H], func=AF.Copy),
                         reads=["ps%d" % pt_], writes=["fx_nb"])
                for b in range(2):
                    col = b * L + L - 1
                    P.op("dve", lambda e, r=r, b=b, ci=ci, col=col: e.scalar_tensor_tensor(out=gsm[:, 2 + b:3 + b], in0=crt[ci][:, col:col + 1],
                                                                                          scalar=cst[:, mlt + r:mlt + r + 1], in1=gsm[:, 2 + b:3 + b], op0=OP.mult, op1=OP.add),
                         reads=["fx_gsm", "cst", "fx_crt%d" % ci], writes=["fx_gsm"])
                    P.op("dve", lambda e, b=b, ci=ci, col=col: e.tensor_tensor(out=gsm[:, b:b + 1], in0=gsm[:, b:b + 1], in1=crt[ci][:, col:col + 1], op=OP.add),
                         reads=["fx_gsm", "fx_crt%d" % ci], writes=["fx_gsm"])
            P.op("act", lambda e: e.dma_start(out=nbd, in_=nb[:].rearrange("p a b c -> p (a b c)")), reads=["fx_nb"], writes=["fx_nbd"], kind="d")
            for b in range(2):
                P.op("dve", lambda e, b=b: e.tensor_scalar(out=cq32[:, b * L:(b + 1) * L], in0=own[:, b * L:(b + 1) * L], scalar1=gsm[:, 2 + b:3 + b], scalar2=None, op0=OP.add),
                     reads=["fx_own", "fx_gsm"], writes=["fx_cq32"])
            P.op("dve", lambda e: e.tensor_copy(out=cqs[:, 0, :], in_=cq32[:]), reads=["fx_cq32"], writes=["fx_cqs0"])
            P.op("dve", lambda e: e.tensor_tensor(out=rem[:], in0=cq32[:], in1=cqs[:, 0, :], op=OP.subtract), reads=["fx_cq32", "fx_cqs0"], writes=["fx_rem"])
            P.op("dve", lambda e: e.tensor_copy(out=cqs[:, 1, :], in_=rem[:]), reads=["fx_rem"], writes=["fx_cqs1"])
            P.op("dve", lambda e: e.tensor_tensor(out=cq32[:], in0=rem[:], in1=cqs[:, 1, :], op=OP.subtract), reads=["fx_rem", "fx_cqs1"], writes=["fx_cq32"])
            P.op("dve", lambda e: e.tensor_copy(out=cqs[:, 2, :], in_=cq32[:]), reads=["fx_cq32"], writes=["fx_cqs2"])
            P.op("act", lambda e: e.dma_start(out=cqd, in_=cqs[:].rearrange("p a b -> p (a b)")), reads=["fx_cqs0", "fx_cqs1", "fx_cqs2"], writes=["fx_cqd"], kind="d")
            areset()
            Ka = ar("fx_Ka", [128, NCORES * L], BF16)
            Va = ar("fx_Va", [128, NCORES * NB, 128], BF16)
            Qa = ar("fx_Qa", [128, L], BF16)
            nb3 = ar("fx_nb3", [128, NCORES, NBT, H], F32)
            caus = ar("fx_caus", [128, TT // 128, TT], F32)
            Pt = [ar("fx_Pt%d" % i, [128, TT], BF16) for i in range(4)]
            tmpx = [ar("fx_tmpx%d" % i, [128, TT], F32) for i in range(2)]
            rsb = [ar("fx_rsb%d" % i, [128, TT], F32) for i in range(2)]
            nsb = ar("fx_nsb", [128, TT], F32)
            ao = ar("fx_ao", [128, TT], BF16)
            P.op("sp", lambda e: e.dma_start(out=nb3[:].rearrange("p a b c -> p (a b c)"), in_=nbd), reads=["fx_nbd"], writes=["fx_nb3"], kind="d")
            for oi in range(TT // 128):
                load_big("negc%d" % (oi * 128), caus[:, oi, :], ("fx_caus", oi))
            P.op("pool", lambda e: e.memset(rsb[0][:], 0.0), writes=["fx_rsb0"])
            P.op("pool", lambda e: e.memset(rsb[1][:], 0.0), writes=["fx_rsb1"])
            P.op("pool", lambda e: e.memset(Ka[64:67, :], 1.0), writes=["fx_Ka1"])
            pcount = 0
            for b in range(2):
                for h in range(H):
                    par = h % 2
                    kc_h = h // 2
                    a_, kcl = kc_h // KA, kc_h % KA
                    r0 = kcl * 128 + par * 64
                    P.op("sp", lambda e, a_=a_, r0=r0, b=b: e.dma_start(
                        out=Ka[0:64, :].rearrange("p (r t) -> p r t", r=NCORES),
                        in_=kall_bf[a_].rearrange("(r x) t -> x r t", r=NCORES)[r0:r0 + 64, :, b * L:(b + 1) * L]),
                        reads=["fx_ka%d" % a_], writes=["fx_Ka"], kind="d")
                    P.op("sp", lambda e, h=h, b=b: e.dma_start(out=Qa[0:64, :], in_=qd[h * 64:(h + 1) * 64, b * L:(b + 1) * L]),
                         reads=["fx_q"], writes=["fx_Qa"], kind="d", semkey="fx_Qa_a")
                    P.op("sp", lambda e, h=h, b=b: e.dma_start(out=Qa[64:67, :], in_=cqd[h:h + 1, :].rearrange("o (c t) -> (o c) t", c=3)[:, b * L:(b + 1) * L]),
                         reads=["fx_cqd"], writes=["fx_Qa2"], kind="d")
                    vo = 0 if par == 0 else 64
                    oo = 64 - vo
                    for rr_ in range(NCORES):
                        P.op("sp", lambda e, h=h, b=b, vo=vo, rr_=rr_: e.dma_start(
                            out=Va[:, rr_ * NB:(rr_ + 1) * NB, vo:vo + 64],
                            in_=vall_bf[b].rearrange("(blk p) d -> p blk d", p=128)[:, rr_ * NB:(rr_ + 1) * NB, h * 64:(h + 1) * 64]),
                            reads=["fx_va%d" % b], writes=[("fx_Va", rr_)], kind="d")
                    P.op("pool", lambda e, oo=oo: e.memset(Va[:, :, oo:oo + 64], 1.0), writes=["fx_Va1"])
                    dlo = 64 if par == 0 else 0
                    nlo = 0 if par == 0 else 64
                    for qi in range(NQ):
                        po = next_ps()
                        ps_reserved.add(po)
                        nblk = NCORES * NB
                        blks = [(r, kb) for r in range(NCORES) for kb in range(NB)]
                        LA = 2
                        pend = {}
                        for it in range(nblk + LA):
                            if it < nblk:
                                r, kb = blks[it]
                                kcol = r * L + kb * 128
                                ps_ = next_ps()
                                P.op("pe", lambda e, ps_=ps_, kcol=kcol, qi=qi: e.matmul(psum[ps_][:, :TT], Ka[0:67, kcol:kcol + 128], Qa[0:67, qi * TT:(qi + 1) * TT],
                                                                                         start=True, stop=True),
                                     reads=["fx_Ka", "fx_Ka1", "fx_Qa", "fx_Qa2"], writes=["ps%d" % ps_])
                                pi_ = pcount % 4
                                pcount += 1
                                src_ap, src_res = psum[ps_][:, :TT], "ps%d" % ps_
                                if kb * 128 >= (qi + 1) * TT:
                                    tx = pcount % 2
                                    P.op("dve", lambda e, ps_=ps_, tx=tx, r=r: e.tensor_scalar(out=tmpx[tx][:], in0=psum[ps_][:, :TT], scalar1=cst[:, nbe + r:nbe + r + 1],
                                                                                           scalar2=None, op0=OP.add),
                                         reads=["ps%d" % ps_, "cst"], writes=[("fx_tmpx", tx)])
                                    src_ap, src_res = tmpx[tx][:], ("fx_tmpx", tx)
                                elif kb * 128 + 127 > qi * TT:
                                    oi = (kb * 128 - qi * TT) // 128
                                    tx = pcount % 2
                                    P.op("dve", lambda e, ps_=ps_, tx=tx, r=r, oi=oi: e.scalar_tensor_tensor(
                                        out=tmpx[tx][:], in0=caus[:, oi, :], scalar=cst[:, meq + r:meq + r + 1], in1=psum[ps_][:, :TT], op0=OP.mult, op1=OP.add),
                                        reads=["ps%d" % ps_, "cst", ("fx_caus", oi)], writes=[("fx_tmpx", tx)])
                                    src_ap, src_res = tmpx[tx][:], ("fx_tmpx", tx)
                                P.op("act", lambda e, src_ap=src_ap, pi_=pi_, r=r, kb=kb, b=b, h=h: e.activation(
                                    out=Pt[pi_][:], in_=src_ap, func=AF.Exp, bias=nb3[:, r, b * NB + kb, h:h + 1]),
                                    reads=[src_res, "fx_nb3"], writes=[("fx_Pt", pi_)])
                                pend[it] = pi_
                            jt = it - LA
                            if jt >= 0:
                                r, kb = blks[jt]
                                pi_ = pend.pop(jt)
                                P.op("pe", lambda e, po=po, r=r, kb=kb, pi_=pi_, jt=jt, nblk=nblk: e.matmul(
                                    psum[po][:, :TT], Va[:, r * NB + kb, :], Pt[pi_][:], start=(jt == 0), stop=(jt == nblk - 1)),
                                    reads=[("fx_Va", r), "fx_Va1", ("fx_Pt", pi_)], writes=["ps%d" % po])
                        P.op("dve", lambda e, po=po, par=par, dlo=dlo: e.reciprocal(out=rsb[par][dlo:dlo + 64, :], in_=psum[po][dlo:dlo + 64, :TT]),
                             reads=["ps%d" % po], writes=["fx_rsb%d" % par])
                        P.op("act", lambda e, po=po, nlo=nlo: e.activation(out=nsb[nlo:nlo + 64, :], in_=psum[po][nlo:nlo + 64, :TT], func=AF.Copy),
                             reads=["ps%d" % po], writes=["fx_nsb"])
                        ps_reserved.discard(po)
                        pw = next_ps()
                        P.op("pe", lambda e, pw=pw, par=par: e.matmul(psum[pw][:, :TT], C("swap"), rsb[par][:], start=True, stop=True),
                             reads=["fx_rsb%d" % par, "cst"], writes=["ps%d" % pw])
                        P.op("dve", lambda e, pw=pw, nlo=nlo: e.tensor_tensor(out=ao[nlo:nlo + 64, :], in0=nsb[nlo:nlo + 64, :], in1=psum[pw][nlo:nlo + 64, :TT], op=OP.mult),
                             reads=["fx_nsb", "ps%d" % pw], writes=["fx_ao"])
                        c0 = b * L + qi * TT
                        P.op("act", lambda e, h=h, nlo=nlo, c0=c0: e.dma_start(out=atd[h * 64:(h + 1) * 64, c0:c0 + TT], in_=ao[nlo:nlo + 64, :]),
                             reads=["fx_ao"], writes=["fx_at"], kind="d")
            areset()
            wstr_alloc()
            att = ar("fx_att", [128, KC, TT], BF16)
            for t in range(NT):
                P.op("sp", lambda e, t=t: e.dma_start(out=att[:], in_=atd.rearrange("(kc p) t -> p kc t", p=128)[:, :, t * TT:(t + 1) * TT]),
                     reads=["fx_at"], writes=["fx_att"], kind="d")
                for dc in range(KC):
                    py = next_ps()
                    proj_fm("fx%dwo" % j, dc, lambda kk: (att[:, kk, :], "fx_att"), KC, TT, py)
                    P.op("act", lambda e, dc=dc, py=py: e.activation(out=ybuf[:, dc, :], in_=psum[py][:, :TT], func=AF.Copy),
                         reads=["ps%d" % py], writes=[("y", dc)])
                residual_ln(l, 0, t, lambda kc: (ybuf[:, kc, :], ("y", kc)))

        nlayers = cfg.DEPTH if cfg.stop is None else cfg.stop
        for l in range(nlayers):
            kind, j = l % 3, l // 3
            if kind == 0:
                gmlp_layer(l, j)
            elif kind == 1:
                if not getattr(cfg, 'nofox', False):
                    fox_layer(l, j)
                else:
                    for t in range(NT):
                        P.op('pool', lambda e: e.memset(ybuf[:], 0.0), writes=[('y', kc) for kc in range(KC)])
                        residual_ln(l, 0, t, lambda kc: (ybuf[:, kc, :], ('y', kc)))
            else:
                pool_layer(l, j)
            if l % 2 == 0:
                dense_ffn_layer(l, l // 2)
            else:
                moe_layer(l, l // 2)

        last = None
        for kc in range(KC):
            for t in range(NT):
                P.op("pool", lambda e, kc=kc, t=t: e.tensor_scalar(out=S[:, kc, t * TT:(t + 1) * TT], in0=S[:, kc, t * TT:(t + 1) * TT],
                                                                    scalar1=1.0 / cfg.alpha, scalar2=None, op0=OP.mult),
                     reads=[("S", kc, t)], writes=[("S", kc, t)])
            last = P.op("act", lambda e, kc=kc: e.dma_start(out=outT[kc * 128:(kc + 1) * 128, :], in_=S[:, kc, :]),
                        reads=[("S", kc, t) for t in range(NT)], writes=["outT"], kind="d", semkey="outT")
        P.final_wait("act", last)
        P.emit(stack)
    return nc


def prepare(cfg, inp):
    flat, nch, widx = pack_weights(cfg, inp)
    in_maps = []
    ncol = 6 * cfg.D // NCORES
    nq = ncol // 128
    fl = flat.reshape(nch, NCORES, PIECE)
    cidx = None
    for c in range(NCORES):
        x0 = inp["x"][0, c * cfg.L:(c + 1) * cfg.L]
        x1 = inp["x"][1, c * cfg.L:(c + 1) * cfg.L]
        xT = np.ascontiguousarray(np.concatenate([x0, x1], 0).T)
        wsh = np.ascontiguousarray(fl[:, c, :]).reshape(nch * 2 * CH_ROWS, CHW)
        mw = np.stack([stat_blocks(inp["mod_w"][l][:, c * ncol:(c + 1) * ncol], cfg.KC) for l in range(cfg.DEPTH)], 0)
        mw = np.ascontiguousarray(mw).reshape(cfg.DEPTH * nq * 128, cfg.KC * 128)
        cst, cidx = pack_consts(cfg, inp, c)
        in_maps.append({"xT": xT, "wsh": wsh, "modw": mw, "cst": np.ascontiguousarray(cst)})
    return in_maps, nch, widx, cidx, in_maps[0]["cst"].shape[1]


def assemble(cfg, results):
    out = np.zeros((2, cfg.SEQ, cfg.D), np.float32)
    for c in range(NCORES):
        o = np.asarray(results[c]["outT"]).T
        out[0, c * cfg.L:(c + 1) * cfg.L] = o[:cfg.L]
        out[1, c * cfg.L:(c + 1) * cfg.L] = o[cfg.L:]
    return out


def kernel(**inputs):
    cfg = Cfg()
    inp = {k: np.asarray(v) for k, v in inputs.items()}
    in_maps, nch, widx, cidx, ncst = prepare(cfg, inp)
    nc = build_program(cfg, nch, widx, cidx, ncst)
    res = run_bass_kernel_spmd(nc, in_maps, core_ids=list(range(NCORES)))
    return assemble(cfg, res.results)
```
